# Optimizing a Trainium2 kernel written in Bass

```python
import math
import jax, jax.numpy as jnp
from jax import lax
import numpy as np

D_MODEL = 1024
BATCH = 8
SEQ = 4096
DEPTH = 4

CHUNK = 64
Q_BLOCK = 128
N_A_LAYERS = DEPTH // 2
N_B_LAYERS = DEPTH - N_A_LAYERS
EPS = 1e-6
NEG = -1e30

A_HEADS = 16
A_HEAD_DIM = D_MODEL // A_HEADS
IDX_HEADS = 8
IDX_DIM = 64
TOPK_MAX = 256
A_IN_COLS = A_HEADS * A_HEAD_DIM + 2 * A_HEAD_DIM + IDX_HEADS * IDX_DIM + IDX_DIM + IDX_HEADS

N_BUCKETS = 32
MAX_DISTANCE = 128

B_HEADS = 16
QK_NOPE = 64
QK_ROPE = 32
QK_DIM = QK_NOPE + QK_ROPE
V_DIM = 64
KV_LORA = 256
Q_LORA = 384
ROPE_THETA = 10000.0

D_FF = 4 * D_MODEL

kernel_name = "yoco_dsa_mla_hybrid_encoder"


def rms_norm(x, g):
    xf = x.astype(jnp.float32)
    y = xf * lax.rsqrt(jnp.mean(xf * xf, axis=-1, keepdims=True) + EPS)
    return (y * g.astype(jnp.float32)).astype(x.dtype)


def t5_bucket(rel):
    nb = N_BUCKETS // 2
    max_exact = nb // 2
    ret = jnp.where(rel > 0, nb, 0)
    n = jnp.abs(rel)
    nf = jnp.maximum(n, 1).astype(jnp.float32)
    large = max_exact + (jnp.log(nf / max_exact) / math.log(MAX_DISTANCE / max_exact)
                         * (nb - max_exact)).astype(jnp.int32)
    large = jnp.minimum(large, nb - 1)
    return ret + jnp.where(n < max_exact, n, large)


def to_blocks(a):
    b, s = a.shape[:2]
    return a.reshape(b, s // Q_BLOCK, Q_BLOCK, *a.shape[2:]).swapaxes(0, 1)


def from_blocks(a):
    nb, b, qb = a.shape[:3]
    return a.swapaxes(0, 1).reshape(b, nb * qb, *a.shape[3:])


def rope_tables(seq_len):
    pos = jnp.arange(seq_len, dtype=jnp.float32)
    inv_freq = 1.0 / (ROPE_THETA ** (jnp.arange(0, QK_ROPE, 2, dtype=jnp.float32) / QK_ROPE))
    ang = pos[:, None] * inv_freq[None, :]
    emb = jnp.concatenate([ang, ang], axis=-1)
    return jnp.cos(emb), jnp.sin(emb)


def rope_tail(x, cos, sin):
    xn, xr = x[..., :QK_NOPE], x[..., QK_NOPE:]
    half = QK_ROPE // 2
    rot = jnp.concatenate([-xr[..., half:], xr[..., :half]], axis=-1)
    xr = xr * cos[:, None, :] + rot * sin[:, None, :]
    return jnp.concatenate([xn, xr.astype(x.dtype)], axis=-1)


def dsa_mixer(h, w_in, q_gain, k_gain, w_o, rel_bias, n_top):
    b, s, _ = h.shape
    proj = h @ w_in
    o1 = A_HEADS * A_HEAD_DIM
    o2 = o1 + A_HEAD_DIM
    o3 = o2 + A_HEAD_DIM
    o4 = o3 + IDX_HEADS * IDX_DIM
    o5 = o4 + IDX_DIM
    q = rms_norm(proj[..., :o1].reshape(b, s, A_HEADS, A_HEAD_DIM), q_gain)
    k = rms_norm(proj[..., o1:o2], k_gain)
    v = proj[..., o2:o3]
    qi = proj[..., o3:o4].reshape(b, s, IDX_HEADS, IDX_DIM)
    ki = proj[..., o4:o5]
    wi = proj[..., o5:]
    key_chunk = jnp.arange(s) // CHUNK

    def block(args):
        qb, qib, wib, t0 = args
        t = t0 + jnp.arange(Q_BLOCK)
        t_chunk = t // CHUNK
        admiss = key_chunk[None, :] <= t_chunk[:, None]
        dots = jnp.einsum('bqhd,bsd->bqhs', qib, ki).astype(jnp.float32) * (IDX_DIM ** -0.5)
        score = jnp.einsum('bqh,bqhs->bqs', wib.astype(jnp.float32) * (IDX_HEADS ** -0.5),
                           jax.nn.relu(dots))
        score = jnp.where(admiss[None], score, NEG)
        _, idx = lax.top_k(score, n_top)
        valid = (idx // CHUNK) <= t_chunk[None, :, None]
        kg = jax.vmap(lambda kk, ii: kk[ii])(k, idx)
        vg = jax.vmap(lambda vv, ii: vv[ii])(v, idx)
        logits = jnp.einsum('bqhd,bqkd->bhqk', qb, kg).astype(jnp.float32) * (A_HEAD_DIM ** -0.5)
        bias = rel_bias[t5_bucket(idx - t[None, :, None])]
        logits = logits + jnp.transpose(bias, (0, 3, 1, 2)).astype(jnp.float32)
        logits = jnp.where(valid[:, None], logits, NEG)
        p = jax.nn.softmax(logits, axis=-1).astype(vg.dtype)
        return jnp.einsum('bhqk,bqkd->bqhd', p, vg)

    starts = jnp.arange(s // Q_BLOCK) * Q_BLOCK
    out = lax.map(block, (to_blocks(q), to_blocks(qi), to_blocks(wi), starts))
    out = from_blocks(out).reshape(b, s, A_HEADS * A_HEAD_DIM)
    return out @ w_o


def mla_shared_kv(h, w_dkv, kv_lora_gain, w_ukv, k_gain, cos, sin):
    b, s, _ = h.shape
    ckr = h @ w_dkv
    c_kv = rms_norm(ckr[..., :KV_LORA], kv_lora_gain)
    k_rope = ckr[..., KV_LORA:]
    kv = (c_kv @ w_ukv).reshape(b, s, B_HEADS, QK_NOPE + V_DIM)
    k_nope, v = kv[..., :QK_NOPE], kv[..., QK_NOPE:]
    k_rope_h = jnp.broadcast_to(k_rope[:, :, None, :], (b, s, B_HEADS, QK_ROPE))
    k = rms_norm(jnp.concatenate([k_nope, k_rope_h], axis=-1), k_gain)
    return rope_tail(k, cos, sin), v


def mla_mixer(h, w_dq, q_lora_gain, w_uq, q_gain, w_o, k, v, cos, sin):
    b, s, _ = h.shape
    q = (rms_norm(h @ w_dq, q_lora_gain) @ w_uq).reshape(b, s, B_HEADS, QK_DIM)
    q = rope_tail(rms_norm(q, q_gain), cos, sin)
    key_chunk = jnp.arange(s) // CHUNK

    def block(args):
        qb, t0 = args
        t = t0 + jnp.arange(Q_BLOCK)
        mask = key_chunk[None, :] <= (t // CHUNK)[:, None]
        logits = jnp.einsum('bqhd,bshd->bhqs', qb, k).astype(jnp.float32) * (QK_DIM ** -0.5)
        logits = jnp.where(mask, logits, NEG)
        p = jax.nn.softmax(logits, axis=-1).astype(v.dtype)
        return jnp.einsum('bhqs,bshd->bqhd', p, v)

    starts = jnp.arange(s // Q_BLOCK) * Q_BLOCK
    out = lax.map(block, (to_blocks(q), starts))
    out = from_blocks(out).reshape(b, s, B_HEADS * V_DIM)
    return out @ w_o


def sq_relu_mlp(h, w_up, w_down):
    return jnp.square(jax.nn.relu(h @ w_up)) @ w_down


def setup_inputs(seed: int = 0) -> dict:
    key = jax.random.key(seed)
    ks = iter(jax.random.split(key, 32))

    def w(shape, fan_in):
        return jax.random.normal(next(ks), shape, jnp.float32) * (fan_in ** -0.5)

    def g(shape):
        return 1.0 + 0.02 * jax.random.normal(next(ks), shape, jnp.float32)

    return {
        "x": jax.random.normal(next(ks), (BATCH, SEQ, D_MODEL), jnp.float32),
        "rel_bias": 0.5 * jax.random.normal(next(ks), (N_BUCKETS, A_HEADS), jnp.float32),
        "a_attn_norm": g((N_A_LAYERS, D_MODEL)),
        "a_w_in": w((N_A_LAYERS, D_MODEL, A_IN_COLS), D_MODEL),
        "a_q_norm": g((N_A_LAYERS, A_HEAD_DIM)),
        "a_k_norm": g((N_A_LAYERS, A_HEAD_DIM)),
        "a_w_o": w((N_A_LAYERS, A_HEADS * A_HEAD_DIM, D_MODEL), A_HEADS * A_HEAD_DIM),
        "kv_norm": g((D_MODEL,)),
        "w_dkv": w((D_MODEL, KV_LORA + QK_ROPE), D_MODEL),
        "kv_lora_norm": g((KV_LORA,)),
        "w_ukv": w((KV_LORA, B_HEADS * (QK_NOPE + V_DIM)), KV_LORA),
        "k_norm": g((QK_DIM,)),
        "b_attn_norm": g((N_B_LAYERS, D_MODEL)),
        "b_w_dq": w((N_B_LAYERS, D_MODEL, Q_LORA), D_MODEL),
        "b_q_lora_norm": g((N_B_LAYERS, Q_LORA)),
        "b_w_uq": w((N_B_LAYERS, Q_LORA, B_HEADS * QK_DIM), Q_LORA),
        "b_q_norm": g((N_B_LAYERS, QK_DIM)),
        "b_w_o": w((N_B_LAYERS, B_HEADS * V_DIM, D_MODEL), B_HEADS * V_DIM),
        "mlp_norm": g((DEPTH, D_MODEL)),
        "mlp_w_up": w((DEPTH, D_MODEL, D_FF), D_MODEL),
        "mlp_w_down": w((DEPTH, D_FF, D_MODEL), D_FF),
    }


def reference(x, rel_bias, a_attn_norm, a_w_in, a_q_norm, a_k_norm, a_w_o,
              kv_norm, w_dkv, kv_lora_norm, w_ukv, k_norm,
              b_attn_norm, b_w_dq, b_q_lora_norm, b_w_uq, b_q_norm, b_w_o,
              mlp_norm, mlp_w_up, mlp_w_down):
    s = x.shape[1]
    n_top = min(TOPK_MAX, s // 4)
    cos, sin = rope_tables(s)
    h = x
    k_shared = None
    v_shared = None
    for layer in range(DEPTH):
        if layer < N_A_LAYERS:
            i = layer
            h = h + dsa_mixer(rms_norm(h, a_attn_norm[i]), a_w_in[i], a_q_norm[i], a_k_norm[i],
                              a_w_o[i], rel_bias, n_top)
        else:
            if layer == N_A_LAYERS:
                k_shared, v_shared = mla_shared_kv(rms_norm(h, kv_norm), w_dkv, kv_lora_norm,
                                                   w_ukv, k_norm, cos, sin)
            j = layer - N_A_LAYERS
            h = h + mla_mixer(rms_norm(h, b_attn_norm[j]), b_w_dq[j], b_q_lora_norm[j], b_w_uq[j],
                              b_q_norm[j], b_w_o[j], k_shared, v_shared, cos, sin)
        h = h + sq_relu_mlp(rms_norm(h, mlp_norm[layer]), mlp_w_up[layer], mlp_w_down[layer])
    return h
```

```python
import math
from contextlib import ExitStack
import numpy as np
import ml_dtypes
import concourse.bass as bass
import concourse.mybir as mybir
from concourse.bass_utils import run_bass_kernel_spmd

F32 = mybir.dt.float32
BF16 = mybir.dt.bfloat16
ALU = mybir.AluOpType
AF = mybir.ActivationFunctionType
AX = mybir.AxisListType

S = 4096
D = 1024
T = 512
NT = S // T
DFF = 4096
EPS = 1e-6
NEGM = -30000.0
NSTEP = 18
WIC = 1864


class Buf:
    __slots__ = ("name", "w", "r")

    def __init__(self, name=""):
        self.name = name
        self.w = None
        self.r = {}


class Op:
    __slots__ = ("eng", "fn", "deps", "sig", "ticket", "dma")

    def __init__(self, eng, fn, dma=False):
        self.eng = eng
        self.fn = fn
        self.deps = []
        self.sig = False
        self.ticket = None
        self.dma = dma


import types


def _freeze(fn, memo=None):
    if memo is None:
        memo = {}
    if not isinstance(fn, types.FunctionType) or fn.__closure__ is None:
        return fn
    if id(fn) in memo:
        return memo[id(fn)]
    cells = []
    for c in fn.__closure__:
        try:
            v = c.cell_contents
        except ValueError:
            cells.append(c)
            continue
        if isinstance(v, types.FunctionType):
            v = _freeze(v, memo)
        cells.append(types.CellType(v))
    nf = types.FunctionType(fn.__code__, fn.__globals__, fn.__name__, fn.__defaults__, tuple(cells))
    nf.__kwdefaults__ = fn.__kwdefaults__
    memo[id(fn)] = nf
    return nf


class Prog:
    def __init__(self, nc, n_dma_sems=40, epoch=30000):
        self.nc = nc
        self.ops = []
        self.last = {}
        self.dma_live = []
        self.n_dma_sems = n_dma_sems
        self.epoch = epoch
        self.engs = {"pe": nc.tensor, "act": nc.scalar, "dve": nc.vector,
                     "pool": nc.gpsimd, "sp": nc.sync}

    def op(self, eng, fn, reads=(), writes=(), dma=False):
        o = Op(eng, _freeze(fn), dma)
        deps = {}
        for b in reads:
            if b.w is not None:
                deps[id(b.w)] = b.w
        for b in writes:
            if b.w is not None:
                if not (eng == "pe" and b.w.eng == "pe" and not b.w.dma):
                    deps[id(b.w)] = b.w
            for re_, ro in b.r.items():
                if re_ == eng and not dma and not ro.dma:
                    continue
                deps[id(ro)] = ro
        for d in deps.values():
            d.sig = True
        o.deps = list(deps.values())
        key = eng if not dma else ("dma", id(o))
        for b in reads:
            b.r[key] = o
        for b in writes:
            b.w = o
            b.r = {}
        self.ops.append(o)
        if dma:
            self.dma_live.append(o)
        else:
            self.last[eng] = o
        return o

    def dma(self, out_ap, in_ap, reads=(), writes=(), q="sp", **kw):
        def fn(e):
            return e.dma_start(out=out_ap, in_=in_ap, **kw)
        return self.op(q, fn, reads, writes, dma=True)

    def barrier(self):
        lasts = list(self.last.values())
        dmas = list(self.dma_live)
        for d in lasts + dmas:
            d.sig = True
        for e in self.engs:
            o = Op(e, None)
            o.deps = [d for d in lasts + dmas if not (d.eng == e and not d.dma)]
            self.ops.append(o)
        self.dma_live = []

    def emit(self):
        nc = self.nc
        engsem = {}
        engcnt = {}
        waited = {e: {} for e in self.engs}
        dsems = [nc.alloc_semaphore(f"dq{i}") for i in range(self.n_dma_sems)]
        dval = [0] * self.n_dma_sems
        dnext = 0
        nsem = 0
        for o in self.ops:
            e = self.engs[o.eng]
            w = waited[o.eng]
            for d in o.deps:
                s, v = d.ticket
                if w.get(id(s), 0) < v:
                    e.wait_ge(s, v)
                    w[id(s)] = v
            if o.fn is None:
                continue
            if o.dma:
                i = dnext
                dnext = (dnext + 1) % self.n_dma_sems
                s = dsems[i]
                if dval[i] > 0 and w.get(id(s), 0) < dval[i]:
                    e.wait_ge(s, dval[i])
                    w[id(s)] = dval[i]
                ins = o.fn(e)
                dval[i] += 16
                ins.then_inc(s, 16)
                o.ticket = (s, dval[i])
            else:
                ins = o.fn(e)
                if o.sig:
                    if o.eng not in engsem or engcnt[o.eng] >= self.epoch:
                        engsem[o.eng] = nc.alloc_semaphore(f"e_{o.eng}_{nsem}")
                        nsem += 1
                        engcnt[o.eng] = 0
                    engcnt[o.eng] += 1
                    ins.then_inc(engsem[o.eng], 1)
                    o.ticket = (engsem[o.eng], engcnt[o.eng])


def _t5_bucket(rel):
    import jax
    import jax.numpy as jnp
    with jax.default_device(jax.devices("cpu")[0]):
        return _t5_bucket_impl(np.asarray(rel), jnp)


def _t5_bucket_impl(rel, jnp):
    rel = jnp.asarray(rel, dtype=jnp.int32)
    nb = 16
    max_exact = 8
    ret = jnp.where(rel > 0, nb, 0)
    n = jnp.abs(rel)
    nf = jnp.maximum(n, 1).astype(jnp.float32)
    large = max_exact + (jnp.log(nf / max_exact) / math.log(128 / max_exact)
                         * (nb - max_exact)).astype(jnp.int32)
    large = jnp.minimum(large, nb - 1)
    return np.asarray(ret + jnp.where(n < max_exact, n, large))


def _consts():
    c = {}
    c["ident"] = np.eye(128, dtype=np.float32)
    blk = np.zeros((128, 128), np.float32)
    blk[:64, :64] = 1.0
    blk[64:, 64:] = 1.0
    c["blk2"] = blk
    j = np.arange(1152)
    bk = _t5_bucket(j - 639)
    oh = np.zeros((32, 1152), np.float32)
    oh[bk, j] = 1.0
    c["oh"] = oh
    k = np.arange(128)[:, None]
    x = np.arange(1024)[None, :]
    s = x // 128
    qp = x % 128
    kpos = 128 * (3 - s) + k
    adm = np.floor_divide(kpos, 64) <= (qp // 64)
    c["mstrip"] = np.where(adm, 0.0, NEGM).astype(np.float32)
    pos = np.arange(S, dtype=np.float32)
    inv = (1.0 / (10000.0 ** (np.arange(0, 32, 2, dtype=np.float32) / 32))).astype(np.float32)
    ang = pos[:, None] * inv[None, :]
    emb = np.concatenate([ang, ang], -1)
    cos = np.cos(emb).astype(np.float32)
    sin = np.sin(emb).astype(np.float32)
    sinm = sin.copy()
    sinm[:, :16] = -sinm[:, :16]
    c["cos"] = cos
    c["sinm"] = sinm
    c["pw"] = np.tile((2.0 ** -(np.arange(NSTEP) + 1.0)).astype(np.float32)[None], (128, 1))
    return c


class _Stop(Exception):
    pass


def build(n_layers=4, stop=None):
    nc = bass.Bass("TRN2", target_bir_lowering=False)
    P = Prog(nc)

    def ckpt(name):
        if stop is not None and name == stop:
            raise _Stop()

    def din(name, shape, dt=F32):
        return nc.dram_tensor(name, list(shape), dt, kind="ExternalInput").ap()

    def dscr(name, shape, dt):
        return nc.dram_tensor(name, list(shape), dt).ap()

    xT = din("xT", [8, 128, S])
    rel_bias = din("rel_bias", [32, 16])
    a_attn_norm = din("a_attn_norm", [2, D])
    a_w_in = din("a_w_in", [2, D, 1736])
    a_q_norm = din("a_q_norm", [2, 64])
    a_k_norm = din("a_k_norm", [2, 64])
    a_w_o = din("a_w_o", [2, D, D])
    kv_norm = din("kv_norm", [D])
    w_dkv = din("w_dkv", [D, 288])
    kv_lora_norm = din("kv_lora_norm", [256])
    w_ukv = din("w_ukv", [256, 2048])
    k_norm = din("k_norm", [96])
    b_attn_norm = din("b_attn_norm", [2, D])
    b_w_dq = din("b_w_dq", [2, D, 384])
    b_q_lora_norm = din("b_q_lora_norm", [2, 384])
    b_w_uq = din("b_w_uq", [2, 384, 1536])
    b_q_norm = din("b_q_norm", [2, 96])
    b_w_o = din("b_w_o", [2, D, D])
    mlp_norm = din("mlp_norm", [4, D])
    mlp_w_up = din("mlp_w_up", [4, D, DFF])
    mlp_w_down = din("mlp_w_down", [4, DFF, D])
    c_ident = din("c_ident", [128, 128])
    c_blk2 = din("c_blk2", [128, 128])
    c_oh = din("c_oh", [32, 1152])
    c_mstrip = din("c_mstrip", [128, 1024])
    c_cos = din("c_cos", [S, 32])
    c_sinm = din("c_sinm", [S, 32])
    c_pw = din("c_pw", [128, NSTEP])
    outT = nc.dram_tensor("outT", [8, 128, S], F32, kind="ExternalOutput").ap()

    Hs = dscr("Hs", [8, 128, S], F32)
    Wi_s = [dscr(f"Wi{l}", [128, 8, WIC], BF16) for l in range(2)]
    Wao_s = [dscr(f"Wao{l}", [128, 8, D], BF16) for l in range(2)]
    Wbo_s = [dscr(f"Wbo{l}", [128, 8, D], BF16) for l in range(2)]
    Wup_s = [dscr(f"Wup{l}", [128, 8, DFF], BF16) for l in range(4)]
    Wdn_s = [dscr(f"Wdn{l}", [128, 32, D], BF16) for l in range(4)]
    Wdkv_s = dscr("Wdkv", [128, 8, 288], BF16)
    Wukv_s = dscr("Wukv", [128, 2, 2048], BF16)
    Wdq_s = [dscr(f"Wdq{l}", [128, 8, 384], BF16) for l in range(2)]
    Wuq_s = [dscr(f"Wuq{l}", [128, 3, 1536], BF16) for l in range(2)]
    G_s = dscr("Gtab", [16, 1152], F32)
    KT_s = dscr("KTs", [16, 96, S], BF16)
    V_s = dscr("Vs", [16, 128, 32, 64], BF16)
    B_Hs = [Buf() for _ in range(NT)]
    B_KT = [Buf() for _ in range(NT)]
    B_V = [Buf() for _ in range(NT)]
    B_G = Buf()

    def sbp(name, shape, dt):
        return nc.alloc_sbuf_tensor(name, list(shape), dt).ap()

    uid = {"i": 0}

    def sbs(es, name, shape, dt):
        uid["i"] += 1
        return es.enter_context(nc.sbuf_tensor(f"{name}_u{uid['i']}", list(shape), dt)).ap()

    banks = [nc.alloc_psum_tensor(f"pb{i}", [128, 512], F32).ap() for i in range(6)]
    bbufs = [Buf(f"pb{i}") for i in range(6)]
    psb0 = nc.alloc_psum_tensor("psb0", [128, 1024], BF16).ap()
    psb1 = nc.alloc_psum_tensor("psb1", [128, 1024], BF16).ap()
    psb_h = [psb0[:, 0:512], psb1[:, 0:512]]
    psb_b = [Buf(), Buf()]
    rot = {"i": 0, "t": 0}

    def ps_short():
        i = 2 + rot["i"] % 4
        rot["i"] += 1
        return banks[i], bbufs[i]

    def ps_tr():
        i = rot["t"] % 2
        rot["t"] += 1
        return psb_h[i], psb_b[i]

    ident = sbp("ident", [128, 128], F32)
    identb = sbp("identb", [128, 128], BF16)
    onesb = sbp("onesb", [128, 128], BF16)
    blk2b = sbp("blk2b", [128, 128], BF16)
    mstrip = sbp("mstrip", [128, 1024], BF16)
    pw = sbp("pw", [128, NSTEP], F32)
    zeros = sbp("zeros", [128, 1], F32)
    epsb = sbp("epsb", [128, 1], F32)
    B_const = Buf("const")
    with ExitStack() as es:
        t0 = sbs(es, "c_t0", [128, 1024], F32)
        bt = Buf()
        P.dma(ident, c_ident, writes=[B_const])
        P.op("dve", lambda e: e.tensor_copy(out=identb, in_=ident), [B_const], [B_const])
        P.dma(t0[:, 0:128], c_blk2, writes=[bt])
        P.op("dve", lambda e: e.tensor_copy(out=blk2b, in_=t0[:, 0:128]), [bt], [B_const, bt])
        P.dma(t0, c_mstrip, reads=[], writes=[bt])
        P.op("dve", lambda e: e.tensor_copy(out=mstrip, in_=t0), [bt], [B_const, bt])
        P.dma(pw, c_pw, writes=[B_const])
        P.op("dve", lambda e: e.memset(onesb, 1.0), [], [B_const])
        P.op("dve", lambda e: e.memset(zeros, 0.0), [], [B_const])
        P.op("dve", lambda e: e.memset(epsb, EPS), [], [B_const])
        P.barrier()

    eng_rr = {"i": 0}
    PREP_ONLY = (stop == "consts")

    def prep(es_bufs, W, gain, dst, KC, segs):
        stage, bstage, sbufs, bbufs_, gcol, gbuf, growT, gbufT = es_bufs
        Dout = W.shape[1]
        DoutS = dst.shape[2]
        if gain is not None:
            P.dma(growT[0:KC, :], gain.rearrange("(kc p) -> kc p", p=128), reads=[], writes=[gbufT])
            pg, pgB = ps_short()
            P.op("pe", lambda e, pg=pg, KC=KC: e.matmul(pg[:, 0:KC], lhsT=growT[0:KC, :], rhs=ident[0:KC, 0:KC], start=True, stop=True),
                 [gbufT, B_const], [pgB])
            P.op("dve", lambda e, pg=pg, KC=KC: e.tensor_copy(out=gcol[:, 0:KC], in_=pg[:, 0:KC]), [pgB], [gbuf])
        for kc in range(KC):
            r = eng_rr["i"] % 5
            eng_rr["i"] += 1
            st, bs, sB, bB = stage[r], bstage[r], sbufs[r], bbufs_[r]
            P.dma(st[:, 0:Dout], W[kc * 128:(kc + 1) * 128, :], writes=[sB])
            eng = ("dve", "act")[eng_rr["i"] % 2]
            for (s0, n, d0) in segs:
                src = st[:, s0:s0 + n]
                dd = bs[:, d0:d0 + n]
                if gain is None:
                    if eng == "act":
                        P.op("act", lambda e, dd=dd, src=src: e.copy(out=dd, in_=src), [sB], [bB])
                    else:
                        P.op(eng, lambda e, dd=dd, src=src: e.tensor_copy(out=dd, in_=src), [sB], [bB])
                else:
                    gc = gcol[:, kc:kc + 1]
                    if eng == "act":
                        P.op("act", lambda e, dd=dd, src=src, gc=gc: e.mul(out=dd, in_=src, mul=gc), [sB, gbuf], [bB])
                    else:
                        P.op(eng, lambda e, dd=dd, src=src, gc=gc: e.tensor_scalar(out=dd, in0=src, scalar1=gc, scalar2=None, op0=ALU.mult),
                             [sB, gbuf], [bB])
            P.dma(dst[:, kc, :], bs[:, 0:DoutS], reads=[bB], writes=[], q="pool")

    with ExitStack() as es:
        stage = [sbs(es, f"pst{i}", [128, 4096], F32) for i in range(5)]
        bstage = [sbs(es, f"pbs{i}", [128, 4096], BF16) for i in range(5)]
        ebufs = (stage, bstage, [Buf() for _ in range(5)], [Buf() for _ in range(5)],
                 sbs(es, "gcol", [128, 32], F32), Buf(), sbs(es, "growT", [32, 128], F32), Buf())
        win_segs = [(0, 1024, 0), (1024, 64, 1024), (1024, 64, 1088), (1664, 64, 1152), (1664, 64, 1216),
                    (1152, 512, 1280), (1088, 64, 1792), (1728, 8, 1856)]
        for l in range(0 if PREP_ONLY else 2):
            prep(ebufs, a_w_in[l], a_attn_norm[l], Wi_s[l], 8, win_segs)
            prep(ebufs, a_w_o[l], None, Wao_s[l], 8, [(0, D, 0)])
            prep(ebufs, b_w_dq[l], b_attn_norm[l], Wdq_s[l], 8, [(0, 384, 0)])
            prep(ebufs, b_w_uq[l], b_q_lora_norm[l], Wuq_s[l], 3, [(0, 1536, 0)])
            prep(ebufs, b_w_o[l], None, Wbo_s[l], 8, [(0, D, 0)])
        prep(ebufs, w_dkv, kv_norm, Wdkv_s, 8, [(0, 288, 0)])
        prep(ebufs, w_ukv, kv_lora_norm, Wukv_s, 2, [(0, 2048, 0)])
        for l in range(0 if PREP_ONLY else 4):
            prep(ebufs, mlp_w_up[l], mlp_norm[l], Wup_s[l], 8, [(0, DFF, 0)])
            prep(ebufs, mlp_w_down[l], None, Wdn_s[l], 32, [(0, D, 0)])
        P.barrier()

    def rms_featmajor(es, src, srcB, KC, Dn, dst, dstB, tag):
        sq = sbs(es, f"sq_{tag}", [128, KC, T], BF16)
        rs = sbs(es, f"rs_{tag}", [128, T], F32)
        Bsq, Brs = Buf(), Buf()
        P.op("act", lambda e: e.activation(out=sq, in_=src, func=AF.Square), [srcB], [Bsq])
        pb, pbB = ps_short()

        def mm(e):
            ins = None
            for kc in range(KC):
                ins = e.matmul(pb, lhsT=onesb, rhs=sq[:, kc, :], start=(kc == 0), stop=(kc == KC - 1))
            return ins
        P.op("pe", mm, [Bsq, B_const], [pbB])
        P.op("dve", lambda e: e.tensor_scalar(out=rs, in0=pb, scalar1=1.0 / Dn, scalar2=EPS, op0=ALU.mult, op1=ALU.add),
             [pbB], [Brs])
        P.op("act", lambda e: e.activation(out=rs, in_=rs, func=AF.Ln), [Brs], [Brs])
        P.op("act", lambda e: e.activation(out=rs, in_=rs, func=AF.Exp, scale=-0.5), [Brs], [Brs])
        P.op("dve", lambda e: e.tensor_tensor(out=dst, in0=src, in1=rs.unsqueeze(1).to_broadcast([128, KC, T]), op=ALU.mult),
             [srcB, Brs], [dstB])

    def load_w(dst, dstB, src):
        P.dma(dst, src, reads=[], writes=[dstB])

    def proj_fm(wt, wB, KC, c0, M, rhs, rhsB):
        pb, pbB = ps_short()

        def mm(e):
            ins = None
            for kc in range(KC):
                ins = e.matmul(pb[0:M, :], lhsT=wt[:, kc, c0:c0 + M], rhs=rhs[:, kc, :], start=(kc == 0), stop=(kc == KC - 1))
            return ins
        P.op("pe", mm, [wB, rhsB], [pbB])
        return pb, pbB

    def mlp_tile(layer, hT, B_h, store=None):
        with ExitStack() as es:
            xn = sbs(es, "m_xn", [128, 8, T], BF16)
            B_xn = Buf()
            uT = sbs(es, "m_uT", [128, 32, T], BF16)
            B_u = [Buf() for _ in range(32)]
            rl = [sbs(es, f"m_rl{i}", [128, T], BF16) for i in range(3)]
            B_rl = [Buf() for _ in range(3)]
            wup = [sbs(es, f"m_wup{i}", [128, 8, 512], BF16) for i in range(2)]
            B_wup = [Buf(), Buf()]
            wdn = [sbs(es, f"m_wdn{i}", [128, 32, 128], BF16) for i in range(2)]
            B_wdn = [Buf(), Buf()]
            load_w(wup[0], B_wup[0], Wup_s[layer][:, :, 0:512])
            rms_featmajor(es, hT, B_h, 8, D, xn, B_xn, "m")
            for g in range(8):
                if g + 1 < 8:
                    load_w(wup[(g + 1) % 2], B_wup[(g + 1) % 2], Wup_s[layer][:, :, (g + 1) * 512:(g + 2) * 512])
                if g == 6:
                    load_w(wdn[0], B_wdn[0], Wdn_s[layer][:, :, 0:128])
                for fi in range(4):
                    f = g * 4 + fi
                    pb, pbB = proj_fm(wup[g % 2], B_wup[g % 2], 8, fi * 128, 128, xn, B_xn)
                    r = f % 3
                    P.op("act", lambda e, pb=pb, r=r: e.activation(out=rl[r], in_=pb, func=AF.Relu), [pbB], [B_rl[r]])
                    eng = "pool" if f % 2 == 0 else "dve"
                    P.op(eng, lambda e, r=r, f=f: e.tensor_tensor(out=uT[:, f, :], in0=rl[r], in1=rl[r], op=ALU.mult),
                         [B_rl[r]], [B_u[f]])
            for o in range(8):
                if o + 1 < 8:
                    load_w(wdn[(o + 1) % 2], B_wdn[(o + 1) % 2], Wdn_s[layer][:, :, (o + 1) * 128:(o + 2) * 128])
                pb, pbB = ps_short()
                w = wdn[o % 2]

                def mm(e, pb=pb, w=w):
                    ins = None
                    for f in range(32):
                        ins = e.matmul(pb, lhsT=w[:, f, :], rhs=uT[:, f, :], start=(f == 0), stop=(f == 31))
                    return ins
                P.op("pe", mm, [B_wdn[o % 2]] + B_u, [pbB])
                P.op("dve", lambda e, pb=pb, o=o: e.tensor_tensor(out=hT[:, o, :], in0=pb, in1=hT[:, o, :], op=ALU.add),
                     [pbB, B_h], [B_h])
            if store is not None:
                store()
            P.barrier()

    acc_rot = {"i": 0}

    def attn_head(nkb, s_mm, exp_args, v_lhsT, vB, PT, B_PT, rsb, B_rs, out_ap, outB, extra_reads, post_mask=None):
        ai = acc_rot["i"] % 2
        acc_rot["i"] += 1
        acc, accB = banks[ai], bbufs[ai]
        sb_list = []

        def issue_s(kb):
            pb, pbB = ps_short()
            P.op("pe", lambda e, pb=pb, kb=kb: s_mm(e, pb, kb), extra_reads(kb), [pbB])
            sb_list.append((pb, pbB))
        issue_s(0)
        if nkb > 1:
            issue_s(1)
        for kb in range(nkb):
            if kb + 2 < nkb:
                issue_s(kb + 2)
            pb, pbB = sb_list[kb]
            r = kb % len(PT)
            bias_ap, scale = exp_args(kb)
            P.op("act", lambda e, pb=pb, r=r, bias_ap=bias_ap, scale=scale:
                 e.activation(out=PT[r], in_=pb, func=AF.Exp, bias=bias_ap, scale=scale),
                 [pbB, B_const], [B_PT[r]])
            if post_mask is not None:
                mk, mkB = post_mask(kb)
                P.op("dve", lambda e, r=r, mk=mk: e.tensor_tensor(out=PT[r], in0=PT[r], in1=mk, op=ALU.mult), [B_PT[r], mkB], [B_PT[r]])
            P.op("pe", lambda e, kb=kb, r=r: e.matmul(acc, lhsT=v_lhsT(kb), rhs=PT[r], start=(kb == 0), stop=(kb == nkb - 1)),
                 [B_PT[r], vB], [accB])
        ri = acc_rot["i"] % 2
        P.op("dve", lambda e: e.reciprocal(out=rsb[ri][64:128, :], in_=acc[64:128, :]), [accB], [B_rs[ri]])
        P.op("dve", lambda e: e.tensor_tensor(out=out_ap, in0=acc[0:64, :], in1=rsb[ri][64:128, :], op=ALU.mult),
             [accB, B_rs[ri]], [outB])

    def oproj_tile(es, Wo, attnT, B_attn, hT, B_h, tag):
        wo = sbs(es, f"wo_{tag}", [128, 8, D], BF16)
        B_wo = Buf()
        load_w(wo, B_wo, Wo)
        for o in range(8):
            pb, pbB = proj_fm(wo, B_wo, 8, o * 128, 128, attnT, B_attn)
            P.op("dve", lambda e, pb=pb, o=o: e.tensor_tensor(out=hT[:, o, :], in0=pb, in1=hT[:, o, :], op=ALU.add),
                 [pbB, B_h], [B_h])

    def dsa_layer(l, es_l, strips, B_strips, cst, first):
        kT2 = sbs(es_l, "kT2", [128, S], BF16)
        kiT2 = sbs(es_l, "kiT2", [128, S], BF16)
        Vaug = sbs(es_l, "Vaug", [128, 32, 128], BF16)
        gq = sbs(es_l, "gq", [128, 2], F32)
        B_k, B_ki, B_V_, B_g = Buf(), Buf(), Buf(), Buf()
        P.op("pool", lambda e: e.memset(Vaug[:, :, 64:128], 1.0), [], [B_V_])
        for hf in range(2):
            P.dma(gq[hf * 64:(hf + 1) * 64, 0:1], a_q_norm[l].rearrange("(p o) -> p o", o=1), writes=[B_g], allow_slow_non_contiguous=True)
            P.dma(gq[hf * 64:(hf + 1) * 64, 1:2], a_k_norm[l].rearrange("(p o) -> p o", o=1), writes=[B_g], allow_slow_non_contiguous=True)
        P.op("dve", lambda e: e.tensor_scalar(out=gq[:, 0:1], in0=gq[:, 0:1], scalar1=0.125, scalar2=None, op0=ALU.mult), [B_g], [B_g])
        CW = 1.0 / (8.0 * math.sqrt(8.0))
        for j in range(NT):
            t0 = j * T
            W = (j + 1) * T
            with ExitStack() as es_t:
                hT = sbs(es_t, "hT", [128, 8, T], F32)
                B_h = Buf()
                src = xT if first else Hs
                P.dma(hT, src.rearrange("kc p t -> p kc t")[:, :, t0:t0 + T], reads=[B_Hs[j]], writes=[B_h])
                es_a = ExitStack()
                qT = sbs(es_a, "qT", [128, 8, T], BF16)
                qiT = sbs(es_a, "qiT", [128, 4, T], BF16)
                wtok = sbs(es_a, "wtok", [128, 4, 8], F32)
                attnT = sbs(es_a, "attnT", [128, 8, T], BF16)
                maskb = sbs(es_a, "maskb", [128, 4, S], BF16)
                B_q, B_qi, B_w, B_attn = Buf(), Buf(), Buf(), Buf()
                B_mb = [Buf() for _ in range(4)]
                with ExitStack() as es:
                    xn = sbs(es, "xn", [128, 8, T], BF16)
                    B_xn = Buf()
                    wi = sbs(es, "wi", [128, 8, WIC], BF16)
                    B_wi = Buf()
                    load_w(wi, B_wi, Wi_s[l])
                    rms_featmajor(es, hT, B_h, 8, D, xn, B_xn, "a")
                    sq2 = [sbs(es, f"sq2_{i}", [128, T], BF16) for i in range(2)]
                    rs2 = [sbs(es, f"rs2_{i}", [128, T], F32) for i in range(2)]
                    B_sq2 = [Buf(), Buf()]
                    B_rs2 = [Buf(), Buf()]
                    for c in range(14):
                        pb, pbB = proj_fm(wi, B_wi, 8, c * 128, 128, xn, B_xn)
                        if c < 9:
                            r = c % 2
                            P.op("act", lambda e, pb=pb, r=r: e.activation(out=sq2[r], in_=pb, func=AF.Square), [pbB], [B_sq2[r]])
                            p2, p2B = ps_short()
                            P.op("pe", lambda e, p2=p2, r=r: e.matmul(p2, lhsT=blk2b, rhs=sq2[r], start=True, stop=True),
                                 [B_sq2[r], B_const], [p2B])
                            P.op("dve", lambda e, p2=p2, r=r: e.tensor_scalar(out=rs2[r], in0=p2, scalar1=1.0 / 64, scalar2=EPS, op0=ALU.mult, op1=ALU.add),
                                 [p2B], [B_rs2[r]])
                            P.op("act", lambda e, r=r: e.activation(out=rs2[r], in_=rs2[r], func=AF.Ln), [B_rs2[r]], [B_rs2[r]])
                            P.op("act", lambda e, r=r: e.activation(out=rs2[r], in_=rs2[r], func=AF.Exp, scale=-0.5), [B_rs2[r]], [B_rs2[r]])
                            if c < 8:
                                dst, dB, gc = qT[:, c, :], B_q, gq[:, 0:1]
                            else:
                                dst, dB, gc = kT2[:, t0:t0 + T], B_k, gq[:, 1:2]
                            P.op("dve", lambda e, pb=pb, r=r, dst=dst, gc=gc: e.scalar_tensor_tensor(out=dst, in0=pb, scalar=gc, in1=rs2[r], op0=ALU.mult, op1=ALU.mult),
                                 [pbB, B_rs2[r], B_g], [dB])
                        elif c == 9:
                            P.op("act", lambda e, pb=pb: e.copy(out=kiT2[:, t0:t0 + T], in_=pb), [pbB], [B_ki])
                        else:
                            P.op("act", lambda e, pb=pb, c=c: e.copy(out=qiT[:, c - 10, :], in_=pb), [pbB], [B_qi])
                    for tb in range(4):
                        pb, pbB = ps_short()

                        def mm(e, pb=pb, tb=tb):
                            ins = None
                            for kc in range(8):
                                ins = e.matmul(pb[:, 0:72], lhsT=xn[:, kc, tb * 128:(tb + 1) * 128], rhs=wi[:, kc, 1792:1864],
                                               start=(kc == 0), stop=(kc == 7))
                            return ins
                        P.op("pe", mm, [B_xn, B_wi], [pbB])
                        P.op("dve", lambda e, pb=pb, tb=tb: e.tensor_copy(out=Vaug[:, 4 * j + tb, 0:64], in_=pb[:, 0:64]), [pbB], [B_V_])
                        P.op("dve", lambda e, pb=pb, tb=tb: e.tensor_copy(out=wtok[:, tb, :], in_=pb[:, 64:72]), [pbB], [B_w])
                    P.barrier()
                ckpt(f"proj{j}")
                with ExitStack() as es:
                    score4 = sbs(es, "score4", [128, 4, S], F32)
                    B_sc = [Buf() for _ in range(4)]
                    Rb = [sbs(es, f"Rb{i}", [128, T], BF16) for i in range(4)]
                    B_R = [Buf() for _ in range(4)]
                    Dg4 = sbs(es, "Dg4", [128, 32, 128], BF16)
                    aw4 = sbs(es, "aw4", [128, 32], F32)
                    sg4 = sbs(es, "sg4", [128, 32], F32)
                    st4 = sbs(es, "st4", [128, 8, 4], F32)
                    thr4 = sbs(es, "thr4", [128, 4], F32)
                    halves4 = sbs(es, "halves4", [128, 4, NSTEP], F32)
                    B_aw, B_Dg, B_lo, B_mid, B_nm, B_cD, B_cA, B_thr = [Buf() for _ in range(8)]
                    wt2 = wtok.rearrange("p i h -> p (i h)")
                    P.op("act", lambda e: e.activation(out=aw4, in_=wt2, func=AF.Abs, scale=CW), [B_w], [B_aw])
                    P.op("act", lambda e: e.activation(out=sg4, in_=wt2, func=AF.Sign), [B_w], [B_aw])
                    P.op("dve", lambda e: e.tensor_tensor(out=Dg4, in0=identb.unsqueeze(1).to_broadcast([128, 32, 128]),
                                                           in1=sg4.unsqueeze(2).to_broadcast([128, 32, 128]), op=ALU.mult),
                         [B_aw, B_const], [B_Dg])
                    P.op("pool", lambda e: e.memset(thr4[:, 0:2], 255.5), [], [B_thr])
                    P.op("pool", lambda e: e.memset(thr4[:, 2:4], 511.0 - W), [], [B_thr])
                    for i in range(4):
                        for kt in range(j + 1):
                            ai = acc_rot["i"] % 2
                            acc_rot["i"] += 1
                            acc, accB = banks[ai], bbufs[ai]
                            dl = []

                            def issue_d(h8, kt=kt, i=i):
                                pb, pbB = ps_short()
                                hp = (h8 % 2) * 64
                                P.op("pe", lambda e, pb=pb: e.matmul(pb, lhsT=qiT[hp:hp + 64, h8 // 2, i * 128:(i + 1) * 128],
                                                                     rhs=kiT2[hp:hp + 64, kt * T:(kt + 1) * T], start=True, stop=True),
                                     [B_qi, B_ki], [pbB])
                                dl.append((pb, pbB))
                            issue_d(0)
                            issue_d(1)
                            for h8 in range(8):
                                if h8 + 2 < 8:
                                    issue_d(h8 + 2)
                                pb, pbB = dl[h8]
                                r = h8 % 4
                                awc = aw4[:, i * 8 + h8:i * 8 + h8 + 1]
                                if h8 % 2 == 0:
                                    P.op("act", lambda e, pb=pb, r=r, awc=awc: e.activation(out=Rb[r], in_=pb, func=AF.Relu, scale=awc),
                                         [pbB, B_aw], [B_R[r]])
                                else:
                                    P.op("dve", lambda e, pb=pb, r=r, awc=awc: e.tensor_scalar(out=Rb[r], in0=pb, scalar1=awc, scalar2=0.0, op0=ALU.mult, op1=ALU.max),
                                         [pbB, B_aw], [B_R[r]])
                                P.op("pe", lambda e, r=r, h8=h8, acc=acc, i=i: e.matmul(acc, lhsT=Dg4[:, i * 8 + h8, :], rhs=Rb[r], start=(h8 == 0), stop=(h8 == 7)),
                                     [B_R[r], B_Dg], [accB])
                            P.op("act", lambda e, acc=acc, kt=kt, i=i: e.copy(out=score4[:, i, kt * T:(kt + 1) * T], in_=acc), [accB], [B_sc[i]])
                    P.op("dve", lambda e: e.tensor_reduce(out=st4[:, 0, :], in_=score4[:, :, 0:W], axis=AX.X, op=ALU.max, apply_absolute_value=True),
                         B_sc, [B_lo])
                    P.op("dve", lambda e: e.tensor_scalar(out=st4[:, 1, :], in0=st4[:, 0, :], scalar1=-1.001, scalar2=-1e-6, op0=ALU.mult, op1=ALU.add),
                         [B_lo], [B_lo])
                    P.op("dve", lambda e: e.tensor_scalar(out=st4[:, 2, :], in0=st4[:, 1, :], scalar1=-2.0, scalar2=None, op0=ALU.mult),
                         [B_lo], [B_lo])
                    P.op("dve", lambda e: e.tensor_tensor(out=halves4, in0=pw.unsqueeze(1).to_broadcast([128, 4, NSTEP]),
                                                           in1=st4[:, 2, :].unsqueeze(2).to_broadcast([128, 4, NSTEP]), op=ALU.mult),
                         [B_lo, B_const], [B_lo])
                    for i in range(4):
                        n_adm = (4 * j + i + 1) * 128
                        if n_adm - 64 < W:
                            P.op("pool", lambda e, n_adm=n_adm, i=i: e.memset(score4[0:64, i, n_adm - 64:W], -1e30), [B_lo], [B_sc[i]])
                        if n_adm < W:
                            P.op("pool", lambda e, n_adm=n_adm, i=i: e.memset(score4[64:128, i, n_adm:W], -1e30), [B_lo], [B_sc[i]])
                    for s_ in range(NSTEP):
                        hs = halves4[:, :, s_]
                        P.op("dve", lambda e, hs=hs: e.tensor_tensor(out=st4[:, 3, :], in0=st4[:, 1, :], in1=hs, op=ALU.add), [B_lo], [B_mid])
                        P.op("dve", lambda e: e.tensor_scalar(out=st4[:, 7, 2:4], in0=st4[:, 3, 2:4], scalar1=-1.0, scalar2=None, op0=ALU.mult),
                             [B_mid], [B_nm])
                        P.op("pool", lambda e: e.memset(st4[:, 4, 2:4], 0.0), [], [B_cA])
                        for i in range(2):
                            P.op("dve", lambda e, i=i: e.tensor_scalar(out=maskb[:, i, 0:W], in0=score4[:, i, 0:W], scalar1=st4[:, 3, i:i + 1], scalar2=None,
                                                                      op0=ALU.is_gt, op1=ALU.add, accum_out=st4[:, 4, i:i + 1]),
                                 [B_sc[i], B_mid], [B_mb[i], B_cD])
                        for i in range(2, 4):
                            P.op("act", lambda e, i=i: e.activation(out=maskb[:, i, 0:W], in_=score4[:, i, 0:W], func=AF.Sign, bias=st4[:, 7, i:i + 1],
                                                                   scale=1.0, accum_out=st4[:, 4, i:i + 1]),
                                 [B_sc[i], B_nm], [B_mb[i], B_cA])
                        P.op("dve", lambda e: e.tensor_tensor(out=st4[:, 5, :], in0=st4[:, 4, :], in1=thr4, op=ALU.is_ge), [B_cD, B_cA, B_thr], [B_lo])
                        P.op("dve", lambda e, hs=hs: e.tensor_tensor(out=st4[:, 6, :], in0=st4[:, 5, :], in1=hs, op=ALU.mult), [B_lo], [B_lo])
                        P.op("dve", lambda e: e.tensor_tensor(out=st4[:, 1, :], in0=st4[:, 1, :], in1=st4[:, 6, :], op=ALU.add), [B_lo], [B_lo])
                    for i in range(4):
                        P.op("dve", lambda e, i=i: e.tensor_scalar(out=maskb[:, i, 0:W], in0=score4[:, i, 0:W], scalar1=st4[:, 1, i:i + 1], scalar2=None,
                                                                  op0=ALU.is_gt),
                             [B_sc[i], B_lo], [B_mb[i]])
                    P.barrier()
                ckpt(f"idx{j}")
                with ExitStack() as es:
                    nkb = 4 * j + 4
                    maskT = sbs(es, "maskT", [128, 32, T], BF16)
                    B_mT = [Buf() for _ in range(32)]
                    PT = [sbs(es, f"PT{i}", [128, T], BF16) for i in range(4)]
                    B_PT = [Buf() for _ in range(4)]
                    rsb = [sbs(es, f"rsb{i}", [128, T], F32) for i in range(2)]
                    B_rs = [Buf(), Buf()]
                    for kb in range(nkb):
                        pt, ptB = ps_tr()

                        def tr(e, pt=pt, kb=kb):
                            ins = None
                            for i in range(4):
                                ins = e.transpose(pt[:, i * 128:(i + 1) * 128], maskb[:, i, kb * 128:(kb + 1) * 128], identb)
                            return ins
                        P.op("pe", tr, B_mb + [B_const], [ptB])
                        import os
                        if os.environ.get("KDBG") == "nocopy":
                            continue
                        eng = "act" if kb % 2 == 0 else "dve"
                        if eng == "act":
                            P.op("act", lambda e, pt=pt, kb=kb: e.copy(out=maskT[:, kb, :], in_=pt), [ptB], [B_mT[kb]])
                        else:
                            P.op("dve", lambda e, pt=pt, kb=kb: e.tensor_copy(out=maskT[:, kb, :], in_=pt), [ptB], [B_mT[kb]])
                    ckpt(f"tr{j}")
                    for h in range(16):
                        ckpt(f"head{j}_{h}")
                        hp = (h % 2) * 64
                        c = h // 2

                        def s_mm(e, pb, kb, hp=hp, c=c, h=h):
                            near = kb >= 4 * j - 1
                            ins = e.matmul(pb, lhsT=kT2[hp:hp + 64, kb * 128:(kb + 1) * 128], rhs=qT[hp:hp + 64, c, :], start=True, stop=(not near))
                            if near:
                                off = (3 - (kb - 4 * j)) * 128
                                ins = e.matmul(pb, lhsT=identb, rhs=strips[:, h, off:off + T], start=False, stop=True)
                            return ins

                        def exp_args(kb, h=h):
                            if kb >= 4 * j - 1:
                                return zeros[:, 0:1], 1.0
                            return cst[:, h:h + 1], 1.0

                        attn_head(nkb, s_mm, exp_args, lambda kb: Vaug[:, kb, :], B_V_, PT, B_PT, rsb, B_rs,
                                  attnT[hp:hp + 64, c, :], B_attn,
                                  lambda kb: [B_k, B_q, B_strips, B_const],
                                  post_mask=lambda kb: (maskT[:, kb, :], B_mT[kb]))
                    P.barrier()
                ckpt(f"attn{j}")
                with ExitStack() as es:
                    oproj_tile(es, Wao_s[l], attnT, B_attn, hT, B_h, "a")
                    P.barrier()
                es_a.close()
                ckpt(f"oproj{j}")
                dstH = outT if (l == n_layers - 1) else Hs
                mlp_tile(l, hT, B_h, store=lambda: P.dma(dstH.rearrange("kc p t -> p kc t")[:, :, t0:t0 + T], hT, reads=[B_h], writes=[B_Hs[j]]))
                ckpt(f"mlp{j}")

    def qk_tmps(es, tag):
        sq = sbs(es, f"qk_sq_{tag}", [128, 16, 96], F32)
        ss = sbs(es, f"qk_ss_{tag}", [128, 16], F32)
        t1 = sbs(es, f"qk_t1_{tag}", [128, 16, 32], F32)
        t2 = sbs(es, f"qk_t2_{tag}", [128, 16, 32], F32)
        return (sq, ss, t1, t2, Buf(), Buf(), Buf(), Buf())

    def qk_finish(tm, full, B_full, gain_b, cs, sn, B_cs, outb, B_out):
        sq, ss, t1, t2, Bs, Bss, Bt1, Bt2 = tm
        P.op("act", lambda e: e.activation(out=sq, in_=full, func=AF.Square), [B_full], [Bs])
        P.op("dve", lambda e: e.tensor_reduce(out=ss, in_=sq, axis=AX.X, op=ALU.add), [Bs], [Bss])
        P.op("dve", lambda e: e.tensor_scalar(out=ss, in0=ss, scalar1=1.0 / 96, scalar2=EPS, op0=ALU.mult, op1=ALU.add), [Bss], [Bss])
        P.op("act", lambda e: e.activation(out=ss, in_=ss, func=AF.Ln), [Bss], [Bss])
        P.op("act", lambda e: e.activation(out=ss, in_=ss, func=AF.Exp, scale=-0.5), [Bss], [Bss])
        P.op("dve", lambda e: e.tensor_tensor(out=full, in0=full, in1=ss.unsqueeze(2).to_broadcast([128, 16, 96]), op=ALU.mult),
             [B_full, Bss], [B_full])
        P.op("dve", lambda e: e.tensor_tensor(out=full, in0=full, in1=gain_b.unsqueeze(1).to_broadcast([128, 16, 96]), op=ALU.mult),
             [B_full, B_const], [B_full])
        P.op("pool", lambda e: e.tensor_copy(out=outb[:, :, 0:64], in_=full[:, :, 0:64]), [B_full], [B_out])
        P.op("dve", lambda e: e.tensor_tensor(out=t1, in0=full[:, :, 64:96], in1=cs.unsqueeze(1).to_broadcast([128, 16, 32]), op=ALU.mult),
             [B_full, B_cs], [Bt1])
        P.op("dve", lambda e: e.tensor_tensor(out=t2[:, :, 0:16], in0=full[:, :, 80:96], in1=sn[:, 0:16].unsqueeze(1).to_broadcast([128, 16, 16]), op=ALU.mult),
             [B_full, B_cs], [Bt2])
        P.op("dve", lambda e: e.tensor_tensor(out=t2[:, :, 16:32], in0=full[:, :, 64:80], in1=sn[:, 16:32].unsqueeze(1).to_broadcast([128, 16, 16]), op=ALU.mult),
             [B_full, B_cs, Bt2], [Bt2])
        P.op("dve", lambda e: e.tensor_tensor(out=outb[:, :, 64:96], in0=t1, in1=t2, op=ALU.add), [Bt1, Bt2, B_out], [B_out])

    def lat_norm(es, src_ps_list, KC, Dn, dst, dstB, tag):
        raw = sbs(es, f"ln_raw_{tag}", [128, KC, T], F32)
        Braw = Buf()
        for kc, (pb, pbB) in enumerate(src_ps_list):
            P.op("act", lambda e, pb=pb, kc=kc: e.copy(out=raw[:, kc, :], in_=pb), [pbB], [Braw])
        rms_featmajor(es, raw, Braw, KC, Dn, dst, dstB, tag)

    def mla_layer(lb, layer, es_l, gen_kv, gk_b, gq_b):
        for j in range(NT):
            t0 = j * T
            with ExitStack() as es_t:
                hT = sbs(es_t, "hT", [128, 8, T], F32)
                B_h = Buf()
                P.dma(hT, Hs.rearrange("kc p t -> p kc t")[:, :, t0:t0 + T], reads=[B_Hs[j]], writes=[B_h])
                qT = sbs(es_t, "b_qT", [128, 16, T], BF16)
                B_qT = Buf()
                attnT = sbs(es_t, "attnT", [128, 8, T], BF16)
                B_attn = Buf()
                cs = sbs(es_t, "cs", [128, 4, 32], F32)
                sn = sbs(es_t, "sn", [128, 4, 32], F32)
                B_cs = Buf()
                P.dma(cs, c_cos[t0:t0 + T, :].rearrange("(tb p) d -> p tb d", p=128), writes=[B_cs])
                P.dma(sn, c_sinm[t0:t0 + T, :].rearrange("(tb p) d -> p tb d", p=128), writes=[B_cs])
                if gen_kv:
                    with ExitStack() as es:
                        xn = sbs(es, "xn", [128, 8, T], BF16)
                        B_xn = Buf()
                        wd = sbs(es, "wdkv", [128, 8, 288], BF16)
                        wu = sbs(es, "wukv", [128, 2, 2048], BF16)
                        B_wd, B_wu = Buf(), Buf()
                        load_w(wd, B_wd, Wdkv_s)
                        load_w(wu, B_wu, Wukv_s)
                        rms_featmajor(es, hT, B_h, 8, D, xn, B_xn, "kv")
                        lat = sbs(es, "lat", [128, 2, T], BF16)
                        B_lat = Buf()
                        pl = [proj_fm(wd, B_wd, 8, c * 128, 128, xn, B_xn) for c in range(2)]
                        lat_norm(es, pl, 2, 256, lat, B_lat, "lat")
                        KTst = sbs(es, "KTst", [128, 16, T], BF16)
                        B_KTst = Buf()
                        kfull = [sbs(es, f"kfull{i}", [128, 16, 96], F32) for i in range(2)]
                        kb16 = [sbs(es, f"kb16{i}", [128, 16, 96], BF16) for i in range(2)]
                        vtok = [sbs(es, f"vtok{i}", [128, 16, 64], BF16) for i in range(2)]
                        B_kf = [Buf(), Buf()]
                        B_kb = [Buf(), Buf()]
                        B_vt = [Buf(), Buf()]
                        ktm = qk_tmps(es, "k")
                        for tb in range(4):
                            r = tb % 2
                            pr, prB = ps_short()

                            def mmr(e, pr=pr, tb=tb):
                                ins = None
                                for kc in range(8):
                                    ins = e.matmul(pr[:, 0:32], lhsT=xn[:, kc, tb * 128:(tb + 1) * 128], rhs=wd[:, kc, 256:288],
                                                   start=(kc == 0), stop=(kc == 7))
                                return ins
                            P.op("pe", mmr, [B_xn, B_wd], [prB])
                            P.op("dve", lambda e, pr=pr, r=r: e.tensor_copy(out=kfull[r][:, :, 64:96], in_=pr[:, 0:32].unsqueeze(1).to_broadcast([128, 16, 32])),
                                 [prB], [B_kf[r]])
                            for cg in range(4):
                                pk, pkB = ps_short()

                                def mmk(e, pk=pk, tb=tb, cg=cg):
                                    ins = None
                                    for c2 in range(2):
                                        ins = e.matmul(pk, lhsT=lat[:, c2, tb * 128:(tb + 1) * 128], rhs=wu[:, c2, cg * 512:(cg + 1) * 512],
                                                       start=(c2 == 0), stop=(c2 == 1))
                                    return ins
                                P.op("pe", mmk, [B_lat, B_wu], [pkB])
                                pk3 = pk.rearrange("p (h d) -> p h d", h=4)
                                if cg % 2 == 0:
                                    P.op("act", lambda e, pk3=pk3, r=r, cg=cg: e.copy(out=kfull[r][:, 4 * cg:4 * cg + 4, 0:64], in_=pk3[:, :, 0:64]),
                                         [pkB], [B_kf[r]])
                                    P.op("act", lambda e, pk3=pk3, r=r, cg=cg: e.copy(out=vtok[r][:, 4 * cg:4 * cg + 4, :], in_=pk3[:, :, 64:128]),
                                         [pkB], [B_vt[r]])
                                else:
                                    P.op("dve", lambda e, pk3=pk3, r=r, cg=cg: e.tensor_copy(out=kfull[r][:, 4 * cg:4 * cg + 4, 0:64], in_=pk3[:, :, 0:64]),
                                         [pkB], [B_kf[r]])
                                    P.op("dve", lambda e, pk3=pk3, r=r, cg=cg: e.tensor_copy(out=vtok[r][:, 4 * cg:4 * cg + 4, :], in_=pk3[:, :, 64:128]),
                                         [pkB], [B_vt[r]])
                            P.dma(V_s[:, :, 4 * j + tb, :].rearrange("h p d -> p h d"), vtok[r], reads=[B_vt[r]], writes=[B_V[j]])
                            qk_finish(ktm, kfull[r], B_kf[r], gk_b, cs[:, tb, :], sn[:, tb, :], B_cs, kb16[r], B_kb[r])
                            for hg in range(4):
                                pt, ptB = ps_tr()

                                def tr(e, pt=pt, r=r, hg=hg):
                                    ins = None
                                    for hh in range(4):
                                        ins = e.transpose(pt[0:96, hh * 128:(hh + 1) * 128], kb16[r][:, 4 * hg + hh, :], identb)
                                    return ins
                                P.op("pe", tr, [B_kb[r], B_const], [ptB])
                                pt3 = pt.rearrange("p (h t) -> p h t", h=4)
                                P.op("act", lambda e, pt3=pt3, hg=hg, tb=tb: e.copy(out=KTst[0:96, 4 * hg:4 * hg + 4, tb * 128:(tb + 1) * 128], in_=pt3[0:96, :, :]),
                                     [ptB], [B_KTst])
                        P.dma(KT_s[:, :, t0:t0 + T].rearrange("h d t -> d h t"), KTst[0:96, :, :], reads=[B_KTst], writes=[B_KT[j]])
                        P.barrier()
                ckpt(f"kv{j}")
                with ExitStack() as es:
                    xn = sbs(es, "xn", [128, 8, T], BF16)
                    B_xn = Buf()
                    wd = sbs(es, "wdq", [128, 8, 384], BF16)
                    wu = sbs(es, "wuq", [128, 3, 1536], BF16)
                    B_wd, B_wu = Buf(), Buf()
                    load_w(wd, B_wd, Wdq_s[lb])
                    load_w(wu, B_wu, Wuq_s[lb])
                    rms_featmajor(es, hT, B_h, 8, D, xn, B_xn, "q")
                    cq = sbs(es, "cq", [128, 3, T], BF16)
                    B_cq = Buf()
                    pl = [proj_fm(wd, B_wd, 8, c * 128, 128, xn, B_xn) for c in range(3)]
                    lat_norm(es, pl, 3, 384, cq, B_cq, "cq")
                    qfull = [sbs(es, f"qfull{i}", [128, 16, 96], F32) for i in range(2)]
                    qb16 = [sbs(es, f"qb16{i}", [128, 16, 96], BF16) for i in range(2)]
                    B_qf = [Buf(), Buf()]
                    B_qb = [Buf(), Buf()]
                    qtm = qk_tmps(es, "q")
                    for tb in range(4):
                        r = tb % 2
                        for cg in range(3):
                            pq, pqB = ps_short()

                            def mmq(e, pq=pq, tb=tb, cg=cg):
                                ins = None
                                for c3 in range(3):
                                    ins = e.matmul(pq, lhsT=cq[:, c3, tb * 128:(tb + 1) * 128], rhs=wu[:, c3, cg * 512:(cg + 1) * 512],
                                                   start=(c3 == 0), stop=(c3 == 2))
                                return ins
                            P.op("pe", mmq, [B_cq, B_wu], [pqB])
                            qf2 = qfull[r].rearrange("p h d -> p (h d)")
                            P.op("act", lambda e, pq=pq, qf2=qf2, cg=cg: e.copy(out=qf2[:, cg * 512:(cg + 1) * 512], in_=pq), [pqB], [B_qf[r]])
                        qk_finish(qtm, qfull[r], B_qf[r], gq_b, cs[:, tb, :], sn[:, tb, :], B_cs, qb16[r], B_qb[r])
                        for hg in range(4):
                            pt, ptB = ps_tr()

                            def tr(e, pt=pt, r=r, hg=hg):
                                ins = None
                                for hh in range(4):
                                    ins = e.transpose(pt[0:96, hh * 128:(hh + 1) * 128], qb16[r][:, 4 * hg + hh, :], identb)
                                return ins
                            P.op("pe", tr, [B_qb[r], B_const], [ptB])
                            pt3 = pt.rearrange("p (h t) -> p h t", h=4)
                            P.op("dve", lambda e, pt3=pt3, hg=hg, tb=tb: e.tensor_copy(out=qT[0:96, 4 * hg:4 * hg + 4, tb * 128:(tb + 1) * 128], in_=pt3[0:96, :, :]),
                                 [ptB], [B_qT])
                    P.barrier()
                ckpt(f"q{j}")
                with ExitStack() as es:
                    nkb = 4 * j + 4
                    nk = nkb * 128
                    KTh = [sbs(es, f"KTh{i}", [128, S], BF16) for i in range(2)]
                    Vh = [sbs(es, f"Vh{i}", [128, 32, 128], BF16) for i in range(2)]
                    B_KTh = [Buf(), Buf()]
                    B_Vh = [Buf(), Buf()]
                    PT = [sbs(es, f"PT{i}", [128, T], BF16) for i in range(4)]
                    B_PT = [Buf() for _ in range(4)]
                    rsb = [sbs(es, f"rsb{i}", [128, T], F32) for i in range(2)]
                    B_rs = [Buf(), Buf()]
                    for i in range(2):
                        P.op("pool", lambda e, i=i: e.memset(Vh[i][:, :, 64:128], 1.0), [], [B_Vh[i]])

                    def load_head(h):
                        r = h % 2
                        P.dma(KTh[r][0:96, 0:nk], KT_s[h, :, 0:nk], reads=B_KT[0:j + 1], writes=[B_KTh[r]])
                        P.dma(Vh[r][:, 0:nkb, 0:64], V_s[h, :, 0:nkb, :], reads=B_V[0:j + 1], writes=[B_Vh[r]])
                    load_head(0)
                    SC = 96 ** -0.5
                    for h in range(16):
                        if h + 1 < 16:
                            load_head(h + 1)
                        r = h % 2
                        hp = (h % 2) * 64
                        c = h // 2

                        def s_mm(e, pb, kb, h=h, r=r):
                            near = kb >= 4 * j
                            ins = e.matmul(pb, lhsT=KTh[r][0:96, kb * 128:(kb + 1) * 128], rhs=qT[0:96, h, :], start=True, stop=(not near))
                            if near:
                                off = (3 - (kb - 4 * j)) * 128
                                ins = e.matmul(pb, lhsT=identb, rhs=mstrip[:, off:off + T], start=False, stop=True)
                            return ins

                        attn_head(nkb, s_mm, lambda kb: (zeros[:, 0:1], SC), lambda kb, r=r: Vh[r][:, kb, :], B_Vh[r], PT, B_PT, rsb, B_rs,
                                  attnT[hp:hp + 64, c, :], B_attn,
                                  lambda kb, r=r: [B_KTh[r], B_qT, B_const])
                    P.barrier()
                ckpt(f"battn{j}")
                with ExitStack() as es:
                    oproj_tile(es, Wbo_s[lb], attnT, B_attn, hT, B_h, "b")
                    P.barrier()
                ckpt(f"boproj{j}")
                last = (layer == n_layers - 1)
                dstH = outT if last else Hs
                mlp_tile(layer, hT, B_h, store=lambda: P.dma(dstH.rearrange("kc p t -> p kc t")[:, :, t0:t0 + T], hT, reads=[B_h], writes=[B_Hs[j]]))

    try:
        nA = min(2, n_layers)
        with ExitStack() as es_dsa:
            strips = sbs(es_dsa, "strips", [128, 16, 1024], BF16)
            cst = sbs(es_dsa, "cst", [128, 16], F32)
            B_strips = Buf()
            with ExitStack() as es:
                rb = sbs(es, "rb", [32, 16], F32)
                oh = sbs(es, "oh", [32, 1152], F32)
                gt = sbs(es, "gt", [16, 1152], F32)
                wn = sbs(es, "wn", [128, 1024], F32)
                mst = sbs(es, "mst", [128, 1024], F32)
                Brb, Bgt, Bwn, Bmst = Buf(), Buf(), Buf(), Buf()
                P.dma(rb, rel_bias, writes=[Brb])
                P.dma(oh, c_oh, writes=[Brb])
                P.dma(mst, c_mstrip, writes=[Bmst])
                for cch in range(3):
                    pb, pbB = ps_short()
                    P.op("pe", lambda e, pb=pb, cch=cch: e.matmul(pb[0:16, 0:384], lhsT=rb, rhs=oh[:, cch * 384:(cch + 1) * 384], start=True, stop=True),
                         [Brb], [pbB])
                    P.op("dve", lambda e, pb=pb, cch=cch: e.tensor_copy(out=gt[:, cch * 384:(cch + 1) * 384], in_=pb[0:16, 0:384]), [pbB], [Bgt])
                P.dma(G_s, gt, reads=[Bgt], writes=[B_G])
                for h in range(16):
                    wap = bass.AP(G_s.tensor, h * 1152, [[1, 128], [1, 1024]])
                    P.dma(wn, wap, reads=[B_G], writes=[Bwn])
                    wrev = bass.AP(wn.tensor, wn.offset + 1023, [list(wn.ap[0]), [-1, 1024]])
                    P.op("dve", lambda e, wrev=wrev, h=h: e.tensor_tensor(out=strips[:, h, :], in0=wrev, in1=mst, op=ALU.add),
                         [Bwn, Bmst], [B_strips])
                    P.op("dve", lambda e, h=h: e.tensor_copy(out=cst[:, h:h + 1], in_=wn[:, 0:1]), [Bwn], [B_strips])
                P.barrier()
            for l in range(nA):
                with ExitStack() as es_l:
                    ckpt("strips")
                    ckpt("consts")
                    dsa_layer(l, es_l, strips, B_strips, cst, first=(l == 0))
                    P.barrier()
            P.barrier()
        if n_layers > 2:
            gk_b = sbp("gk_b", [128, 96], F32)
            gq_bs = [sbp(f"gq_b{i}", [128, 96], F32) for i in range(2)]
            P.dma(gk_b, bass.AP(k_norm.tensor, 0, [[0, 128], [1, 96]]), writes=[B_const])
            for i in range(2):
                P.dma(gq_bs[i], bass.AP(b_q_norm.tensor, i * 96, [[0, 128], [1, 96]]), writes=[B_const])
            for lb in range(n_layers - 2):
                with ExitStack() as es_l:
                    mla_layer(lb, 2 + lb, es_l, gen_kv=(lb == 0), gk_b=gk_b, gq_b=gq_bs[lb])
                    P.barrier()
    except _Stop:
        pass
    P.barrier()
    P.emit()
    return nc


_CACHE = {}


def kernel(**inputs):
    n_layers = 4
    if "nc" not in _CACHE:
        _CACHE["nc"] = build(n_layers)
        _CACHE["consts"] = _consts()
    nc = _CACHE["nc"]
    cs = _CACHE["consts"]
    x = np.asarray(inputs["x"], dtype=np.float32)
    shared = {k: np.ascontiguousarray(np.asarray(v, dtype=np.float32)) for k, v in inputs.items() if k != "x"}
    shared.update({"c_ident": cs["ident"], "c_blk2": cs["blk2"], "c_oh": cs["oh"], "c_mstrip": cs["mstrip"],
                   "c_cos": cs["cos"], "c_sinm": cs["sinm"], "c_pw": cs["pw"]})
    in_maps = []
    for b in range(8):
        m = dict(shared)
        m["xT"] = np.ascontiguousarray(x[b].T).reshape(8, 128, S)
        in_maps.append(m)
    res = run_bass_kernel_spmd(nc, in_maps, core_ids=list(range(8)))
    out = np.stack([np.ascontiguousarray(r["outT"].reshape(D, S).T) for r in res.results], 0)
    return out.astype(np.float32)
```

```python
import math
from contextlib import ExitStack
import numpy as np
import ml_dtypes
import concourse.bass as bass
import concourse.mybir as mybir
from concourse.bass_utils import run_bass_kernel_spmd

F32 = mybir.dt.float32
BF16 = mybir.dt.bfloat16
ALU = mybir.AluOpType
AF = mybir.ActivationFunctionType
AX = mybir.AxisListType

S = 4096
D = 1024
T = 512
NT = S // T
DFF = 4096
EPS = 1e-6
NEGM = -30000.0
NSTEP = 18
WIC = 1864


class Buf:
    __slots__ = ("name", "w", "r")

    def __init__(self, name=""):
        self.name = name
        self.w = None
        self.r = {}


class Op:
    __slots__ = ("eng", "fn", "deps", "sig", "ticket", "dma")

    def __init__(self, eng, fn, dma=False):
        self.eng = eng
        self.fn = fn
        self.deps = []
        self.sig = False
        self.ticket = None
        self.dma = dma


import types


def _freeze(fn, memo=None):
    if memo is None:
        memo = {}
    if not isinstance(fn, types.FunctionType) or fn.__closure__ is None:
        return fn
    if id(fn) in memo:
        return memo[id(fn)]
    cells = []
    for c in fn.__closure__:
        try:
            v = c.cell_contents
        except ValueError:
            cells.append(c)
            continue
        if isinstance(v, types.FunctionType):
            v = _freeze(v, memo)
        cells.append(types.CellType(v))
    nf = types.FunctionType(fn.__code__, fn.__globals__, fn.__name__, fn.__defaults__, tuple(cells))
    nf.__kwdefaults__ = fn.__kwdefaults__
    memo[id(fn)] = nf
    return nf


class Prog:
    def __init__(self, nc, n_dma_sems=40, epoch=30000):
        self.nc = nc
        self.ops = []
        self.last = {}
        self.dma_live = []
        self.n_dma_sems = n_dma_sems
        self.epoch = epoch
        self.engs = {"pe": nc.tensor, "act": nc.scalar, "dve": nc.vector,
                     "pool": nc.gpsimd, "sp": nc.sync}

    def op(self, eng, fn, reads=(), writes=(), dma=False):
        o = Op(eng, _freeze(fn), dma)
        deps = {}
        for b in reads:
            if b.w is not None:
                deps[id(b.w)] = b.w
        for b in writes:
            if b.w is not None:
                if not (eng == "pe" and b.w.eng == "pe" and not b.w.dma):
                    deps[id(b.w)] = b.w
            for re_, ro in b.r.items():
                if re_ == eng and not dma and not ro.dma:
                    continue
                deps[id(ro)] = ro
        for d in deps.values():
            d.sig = True
        o.deps = list(deps.values())
        key = eng if not dma else ("dma", id(o))
        for b in reads:
            b.r[key] = o
        for b in writes:
            b.w = o
            b.r = {}
        self.ops.append(o)
        if dma:
            self.dma_live.append(o)
        else:
            self.last[eng] = o
        return o

    def dma(self, out_ap, in_ap, reads=(), writes=(), q="sp", **kw):
        def fn(e):
            return e.dma_start(out=out_ap, in_=in_ap, **kw)
        return self.op(q, fn, reads, writes, dma=True)

    def barrier(self):
        lasts = list(self.last.values())
        dmas = list(self.dma_live)
        for d in lasts + dmas:
            d.sig = True
        for e in self.engs:
            o = Op(e, None)
            o.deps = [d for d in lasts + dmas if not (d.eng == e and not d.dma)]
            self.ops.append(o)
        self.dma_live = []

    def emit(self):
        nc = self.nc
        engsem = {}
        engcnt = {}
        waited = {e: {} for e in self.engs}
        dsems = [nc.alloc_semaphore(f"dq{i}") for i in range(self.n_dma_sems)]
        dval = [0] * self.n_dma_sems
        dnext = 0
        nsem = 0
        for o in self.ops:
            e = self.engs[o.eng]
            w = waited[o.eng]
            for d in o.deps:
                s, v = d.ticket
                if w.get(id(s), 0) < v:
                    e.wait_ge(s, v)
                    w[id(s)] = v
            if o.fn is None:
                continue
            if o.dma:
                i = dnext
                dnext = (dnext + 1) % self.n_dma_sems
                s = dsems[i]
                if dval[i] > 0 and w.get(id(s), 0) < dval[i]:
                    e.wait_ge(s, dval[i])
                    w[id(s)] = dval[i]
                ins = o.fn(e)
                dval[i] += 16
                ins.then_inc(s, 16)
                o.ticket = (s, dval[i])
            else:
                ins = o.fn(e)
                if o.sig:
                    if o.eng not in engsem or engcnt[o.eng] >= self.epoch:
                        engsem[o.eng] = nc.alloc_semaphore(f"e_{o.eng}_{nsem}")
                        nsem += 1
                        engcnt[o.eng] = 0
                    engcnt[o.eng] += 1
                    ins.then_inc(engsem[o.eng], 1)
                    o.ticket = (engsem[o.eng], engcnt[o.eng])


def _t5_bucket(rel):
    import jax
    import jax.numpy as jnp
    with jax.default_device(jax.devices("cpu")[0]):
        return _t5_bucket_impl(np.asarray(rel), jnp)


def _t5_bucket_impl(rel, jnp):
    rel = jnp.asarray(rel, dtype=jnp.int32)
    nb = 16
    max_exact = 8
    ret = jnp.where(rel > 0, nb, 0)
    n = jnp.abs(rel)
    nf = jnp.maximum(n, 1).astype(jnp.float32)
    large = max_exact + (jnp.log(nf / max_exact) / math.log(128 / max_exact)
                         * (nb - max_exact)).astype(jnp.int32)
    large = jnp.minimum(large, nb - 1)
    return np.asarray(ret + jnp.where(n < max_exact, n, large))


def _consts():
    c = {}
    c["ident"] = np.eye(128, dtype=np.float32)
    blk = np.zeros((128, 128), np.float32)
    blk[:64, :64] = 1.0
    blk[64:, 64:] = 1.0
    c["blk2"] = blk
    j = np.arange(1152)
    bk = _t5_bucket(j - 639)
    oh = np.zeros((32, 1152), np.float32)
    oh[bk, j] = 1.0
    c["oh"] = oh
    k = np.arange(128)[:, None]
    x = np.arange(1024)[None, :]
    s = x // 128
    qp = x % 128
    kpos = 128 * (3 - s) + k
    adm = np.floor_divide(kpos, 64) <= (qp // 64)
    c["mstrip"] = np.where(adm, 0.0, NEGM).astype(np.float32)
    pos = np.arange(S, dtype=np.float32)
    inv = (1.0 / (10000.0 ** (np.arange(0, 32, 2, dtype=np.float32) / 32))).astype(np.float32)
    ang = pos[:, None] * inv[None, :]
    emb = np.concatenate([ang, ang], -1)
    cos = np.cos(emb).astype(np.float32)
    sin = np.sin(emb).astype(np.float32)
    sinm = sin.copy()
    sinm[:, :16] = -sinm[:, :16]
    c["cos"] = cos
    c["sinm"] = sinm
    c["pw"] = np.tile((2.0 ** -(np.arange(NSTEP) + 1.0)).astype(np.float32)[None], (128, 1))
    return c


class _Stop(Exception):
    pass


def build(n_layers=4, stop=None):
    nc = bass.Bass("TRN2", target_bir_lowering=False)
    P = Prog(nc)

    def ckpt(name):
        if stop is not None and name == stop:
            raise _Stop()

    def din(name, shape, dt=F32):
        return nc.dram_tensor(name, list(shape), dt, kind="ExternalInput").ap()

    def dscr(name, shape, dt):
        return nc.dram_tensor(name, list(shape), dt).ap()

    xT = din("xT", [8, 128, S])
    rel_bias = din("rel_bias", [32, 16])
    a_attn_norm = din("a_attn_norm", [2, D])
    a_w_in = din("a_w_in", [2, D, 1736])
    a_q_norm = din("a_q_norm", [2, 64])
    a_k_norm = din("a_k_norm", [2, 64])
    a_w_o = din("a_w_o", [2, D, D])
    kv_norm = din("kv_norm", [D])
    w_dkv = din("w_dkv", [D, 288])
    kv_lora_norm = din("kv_lora_norm", [256])
    w_ukv = din("w_ukv", [256, 2048])
    k_norm = din("k_norm", [96])
    b_attn_norm = din("b_attn_norm", [2, D])
    b_w_dq = din("b_w_dq", [2, D, 384])
    b_q_lora_norm = din("b_q_lora_norm", [2, 384])
    b_w_uq = din("b_w_uq", [2, 384, 1536])
    b_q_norm = din("b_q_norm", [2, 96])
    b_w_o = din("b_w_o", [2, D, D])
    mlp_norm = din("mlp_norm", [4, D])
    mlp_w_up = din("mlp_w_up", [4, D, DFF])
    mlp_w_down = din("mlp_w_down", [4, DFF, D])
    c_ident = din("c_ident", [128, 128])
    c_blk2 = din("c_blk2", [128, 128])
    c_oh = din("c_oh", [32, 1152])
    c_mstrip = din("c_mstrip", [128, 1024])
    c_cos = din("c_cos", [S, 32])
    c_sinm = din("c_sinm", [S, 32])
    c_pw = din("c_pw", [128, NSTEP])
    outT = nc.dram_tensor("outT", [8, 128, S], F32, kind="ExternalOutput").ap()

    Hs = dscr("Hs", [8, 128, S], F32)
    Wi_s = [dscr(f"Wi{l}", [128, 8, WIC], BF16) for l in range(2)]
    Wao_s = [dscr(f"Wao{l}", [128, 8, D], BF16) for l in range(2)]
    Wbo_s = [dscr(f"Wbo{l}", [128, 8, D], BF16) for l in range(2)]
    Wup_s = [dscr(f"Wup{l}", [128, 8, DFF], BF16) for l in range(4)]
    Wdn_s = [dscr(f"Wdn{l}", [128, 32, D], BF16) for l in range(4)]
    Wdkv_s = dscr("Wdkv", [128, 8, 288], BF16)
    Wukv_s = dscr("Wukv", [128, 2, 2048], BF16)
    Wdq_s = [dscr(f"Wdq{l}", [128, 8, 384], BF16) for l in range(2)]
    Wuq_s = [dscr(f"Wuq{l}", [128, 3, 1536], BF16) for l in range(2)]
    G_s = dscr("Gtab", [16, 1152], F32)
    KT_s = dscr("KTs", [16, 96, S], BF16)
    V_s = dscr("Vs", [16, 128, 32, 64], BF16)
    B_Hs = [Buf() for _ in range(NT)]
    B_KT = [Buf() for _ in range(NT)]
    B_V = [Buf() for _ in range(NT)]
    B_G = Buf()

    def sbp(name, shape, dt):
        return nc.alloc_sbuf_tensor(name, list(shape), dt).ap()

    uid = {"i": 0}

    def sbs(es, name, shape, dt):
        uid["i"] += 1
        return es.enter_context(nc.sbuf_tensor(f"{name}_u{uid['i']}", list(shape), dt)).ap()

    banks = [nc.alloc_psum_tensor(f"pb{i}", [128, 512], F32).ap() for i in range(6)]
    bbufs = [Buf(f"pb{i}") for i in range(6)]
    psb0 = nc.alloc_psum_tensor("psb0", [128, 1024], BF16).ap()
    psb1 = nc.alloc_psum_tensor("psb1", [128, 1024], BF16).ap()
    psb_h = [psb0[:, 0:512], psb1[:, 0:512]]
    psb_b = [Buf(), Buf()]
    rot = {"i": 0, "t": 0}

    def ps_short():
        i = 2 + rot["i"] % 4
        rot["i"] += 1
        return banks[i], bbufs[i]

    def ps_tr():
        i = rot["t"] % 2
        rot["t"] += 1
        return psb_h[i], psb_b[i]

    ident = sbp("ident", [128, 128], F32)
    identb = sbp("identb", [128, 128], BF16)
    onesb = sbp("onesb", [128, 128], BF16)
    blk2b = sbp("blk2b", [128, 128], BF16)
    mstrip = sbp("mstrip", [128, 1024], BF16)
    pw = sbp("pw", [128, NSTEP], F32)
    zeros = sbp("zeros", [128, 1], F32)
    epsb = sbp("epsb", [128, 1], F32)
    B_const = Buf("const")
    with ExitStack() as es:
        t0 = sbs(es, "c_t0", [128, 1024], F32)
        bt = Buf()
        P.dma(ident, c_ident, writes=[B_const])
        P.op("dve", lambda e: e.tensor_copy(out=identb, in_=ident), [B_const], [B_const])
        P.dma(t0[:, 0:128], c_blk2, writes=[bt])
        P.op("dve", lambda e: e.tensor_copy(out=blk2b, in_=t0[:, 0:128]), [bt], [B_const, bt])
        P.dma(t0, c_mstrip, reads=[], writes=[bt])
        P.op("dve", lambda e: e.tensor_copy(out=mstrip, in_=t0), [bt], [B_const, bt])
        P.dma(pw, c_pw, writes=[B_const])
        P.op("dve", lambda e: e.memset(onesb, 1.0), [], [B_const])
        P.op("dve", lambda e: e.memset(zeros, 0.0), [], [B_const])
        P.op("dve", lambda e: e.memset(epsb, EPS), [], [B_const])
        P.barrier()

    eng_rr = {"i": 0}
    PREP_ONLY = (stop == "consts")

    def prep(es_bufs, W, gain, dst, KC, segs):
        stage, bstage, sbufs, bbufs_, gcol, gbuf, growT, gbufT = es_bufs
        Dout = W.shape[1]
        DoutS = dst.shape[2]
        if gain is not None:
            P.dma(growT[0:KC, :], gain.rearrange("(kc p) -> kc p", p=128), reads=[], writes=[gbufT])
            pg, pgB = ps_short()
            P.op("pe", lambda e, pg=pg, KC=KC: e.matmul(pg[:, 0:KC], lhsT=growT[0:KC, :], rhs=ident[0:KC, 0:KC], start=True, stop=True),
                 [gbufT, B_const], [pgB])
            P.op("dve", lambda e, pg=pg, KC=KC: e.tensor_copy(out=gcol[:, 0:KC], in_=pg[:, 0:KC]), [pgB], [gbuf])
        for kc in range(KC):
            r = eng_rr["i"] % 5
            eng_rr["i"] += 1
            st, bs, sB, bB = stage[r], bstage[r], sbufs[r], bbufs_[r]
            P.dma(st[:, 0:Dout], W[kc * 128:(kc + 1) * 128, :], writes=[sB])
            eng = ("dve", "act")[eng_rr["i"] % 2]
            for (s0, n, d0) in segs:
                src = st[:, s0:s0 + n]
                dd = bs[:, d0:d0 + n]
                if gain is None:
                    if eng == "act":
                        P.op("act", lambda e, dd=dd, src=src: e.copy(out=dd, in_=src), [sB], [bB])
                    else:
                        P.op(eng, lambda e, dd=dd, src=src: e.tensor_copy(out=dd, in_=src), [sB], [bB])
                else:
                    gc = gcol[:, kc:kc + 1]
                    if eng == "act":
                        P.op("act", lambda e, dd=dd, src=src, gc=gc: e.mul(out=dd, in_=src, mul=gc), [sB, gbuf], [bB])
                    else:
                        P.op(eng, lambda e, dd=dd, src=src, gc=gc: e.tensor_scalar(out=dd, in0=src, scalar1=gc, scalar2=None, op0=ALU.mult),
                             [sB, gbuf], [bB])
            P.dma(dst[:, kc, :], bs[:, 0:DoutS], reads=[bB], writes=[], q="pool")

    with ExitStack() as es:
        stage = [sbs(es, f"pst{i}", [128, 4096], F32) for i in range(5)]
        bstage = [sbs(es, f"pbs{i}", [128, 4096], BF16) for i in range(5)]
        ebufs = (stage, bstage, [Buf() for _ in range(5)], [Buf() for _ in range(5)],
                 sbs(es, "gcol", [128, 32], F32), Buf(), sbs(es, "growT", [32, 128], F32), Buf())
        win_segs = [(0, 1024, 0), (1024, 64, 1024), (1024, 64, 1088), (1664, 64, 1152), (1664, 64, 1216),
                    (1152, 512, 1280), (1088, 64, 1792), (1728, 8, 1856)]
        for l in range(0 if PREP_ONLY else 2):
            prep(ebufs, a_w_in[l], a_attn_norm[l], Wi_s[l], 8, win_segs)
            prep(ebufs, a_w_o[l], None, Wao_s[l], 8, [(0, D, 0)])
            prep(ebufs, b_w_dq[l], b_attn_norm[l], Wdq_s[l], 8, [(0, 384, 0)])
            prep(ebufs, b_w_uq[l], b_q_lora_norm[l], Wuq_s[l], 3, [(0, 1536, 0)])
            prep(ebufs, b_w_o[l], None, Wbo_s[l], 8, [(0, D, 0)])
        prep(ebufs, w_dkv, kv_norm, Wdkv_s, 8, [(0, 288, 0)])
        prep(ebufs, w_ukv, kv_lora_norm, Wukv_s, 2, [(0, 2048, 0)])
        for l in range(0 if PREP_ONLY else 4):
            prep(ebufs, mlp_w_up[l], mlp_norm[l], Wup_s[l], 8, [(0, DFF, 0)])
            prep(ebufs, mlp_w_down[l], None, Wdn_s[l], 32, [(0, D, 0)])
        P.barrier()

    def rms_featmajor(es, src, srcB, KC, Dn, dst, dstB, tag):
        sq = sbs(es, f"sq_{tag}", [128, KC, T], BF16)
        rs = sbs(es, f"rs_{tag}", [128, T], F32)
        Bsq, Brs = Buf(), Buf()
        P.op("act", lambda e: e.activation(out=sq, in_=src, func=AF.Square), [srcB], [Bsq])
        pb, pbB = ps_short()

        def mm(e):
            ins = None
            for kc in range(KC):
                ins = e.matmul(pb, lhsT=onesb, rhs=sq[:, kc, :], start=(kc == 0), stop=(kc == KC - 1))
            return ins
        P.op("pe", mm, [Bsq, B_const], [pbB])
        P.op("dve", lambda e: e.tensor_scalar(out=rs, in0=pb, scalar1=1.0 / Dn, scalar2=EPS, op0=ALU.mult, op1=ALU.add),
             [pbB], [Brs])
        P.op("act", lambda e: e.activation(out=rs, in_=rs, func=AF.Ln), [Brs], [Brs])
        P.op("act", lambda e: e.activation(out=rs, in_=rs, func=AF.Exp, scale=-0.5), [Brs], [Brs])
        P.op("dve", lambda e: e.tensor_tensor(out=dst, in0=src, in1=rs.unsqueeze(1).to_broadcast([128, KC, T]), op=ALU.mult),
             [srcB, Brs], [dstB])

    def load_w(dst, dstB, src):
        P.dma(dst, src, reads=[], writes=[dstB])

    def proj_fm(wt, wB, KC, c0, M, rhs, rhsB):
        pb, pbB = ps_short()

        def mm(e):
            ins = None
            for kc in range(KC):
                ins = e.matmul(pb[0:M, :], lhsT=wt[:, kc, c0:c0 + M], rhs=rhs[:, kc, :], start=(kc == 0), stop=(kc == KC - 1))
            return ins
        P.op("pe", mm, [wB, rhsB], [pbB])
        return pb, pbB

    def mlp_tile(layer, hT, B_h, store=None):
        with ExitStack() as es:
            xn = sbs(es, "m_xn", [128, 8, T], BF16)
            B_xn = Buf()
            uT = sbs(es, "m_uT", [128, 32, T], BF16)
            B_u = [Buf() for _ in range(32)]
            rl = [sbs(es, f"m_rl{i}", [128, T], BF16) for i in range(3)]
            B_rl = [Buf() for _ in range(3)]
            wup = [sbs(es, f"m_wup{i}", [128, 8, 512], BF16) for i in range(2)]
            B_wup = [Buf(), Buf()]
            wdn = [sbs(es, f"m_wdn{i}", [128, 32, 128], BF16) for i in range(2)]
            B_wdn = [Buf(), Buf()]
            load_w(wup[0], B_wup[0], Wup_s[layer][:, :, 0:512])
            rms_featmajor(es, hT, B_h, 8, D, xn, B_xn, "m")
            for g in range(8):
                if g + 1 < 8:
                    load_w(wup[(g + 1) % 2], B_wup[(g + 1) % 2], Wup_s[layer][:, :, (g + 1) * 512:(g + 2) * 512])
                if g == 6:
                    load_w(wdn[0], B_wdn[0], Wdn_s[layer][:, :, 0:128])
                for fi in range(4):
                    f = g * 4 + fi
                    pb, pbB = proj_fm(wup[g % 2], B_wup[g % 2], 8, fi * 128, 128, xn, B_xn)
                    r = f % 3
                    P.op("act", lambda e, pb=pb, r=r: e.activation(out=rl[r], in_=pb, func=AF.Relu), [pbB], [B_rl[r]])
                    eng = "pool" if f % 2 == 0 else "dve"
                    P.op(eng, lambda e, r=r, f=f: e.tensor_tensor(out=uT[:, f, :], in0=rl[r], in1=rl[r], op=ALU.mult),
                         [B_rl[r]], [B_u[f]])
            for o in range(8):
                if o + 1 < 8:
                    load_w(wdn[(o + 1) % 2], B_wdn[(o + 1) % 2], Wdn_s[layer][:, :, (o + 1) * 128:(o + 2) * 128])
                pb, pbB = ps_short()
                w = wdn[o % 2]

                def mm(e, pb=pb, w=w):
                    ins = None
                    for f in range(32):
                        ins = e.matmul(pb, lhsT=w[:, f, :], rhs=uT[:, f, :], start=(f == 0), stop=(f == 31))
                    return ins
                P.op("pe", mm, [B_wdn[o % 2]] + B_u, [pbB])
                P.op("dve", lambda e, pb=pb, o=o: e.tensor_tensor(out=hT[:, o, :], in0=pb, in1=hT[:, o, :], op=ALU.add),
                     [pbB, B_h], [B_h])
            if store is not None:
                store()
            P.barrier()

    acc_rot = {"i": 0}

    def attn_head(nkb, s_mm, exp_args, v_lhsT, vB, PT, B_PT, rsb, B_rs, out_ap, outB, extra_reads, post_mask=None):
        ai = acc_rot["i"] % 2
        acc_rot["i"] += 1
        acc, accB = banks[ai], bbufs[ai]
        sb_list = []

        def issue_s(kb):
            pb, pbB = ps_short()
            P.op("pe", lambda e, pb=pb, kb=kb: s_mm(e, pb, kb), extra_reads(kb), [pbB])
            sb_list.append((pb, pbB))
        issue_s(0)
        if nkb > 1:
            issue_s(1)
        for kb in range(nkb):
            if kb + 2 < nkb:
                issue_s(kb + 2)
            pb, pbB = sb_list[kb]
            r = kb % len(PT)
            bias_ap, scale = exp_args(kb)
            P.op("act", lambda e, pb=pb, r=r, bias_ap=bias_ap, scale=scale:
                 e.activation(out=PT[r], in_=pb, func=AF.Exp, bias=bias_ap, scale=scale),
                 [pbB, B_const], [B_PT[r]])
            if post_mask is not None:
                mk, mkB = post_mask(kb)
                P.op("dve", lambda e, r=r, mk=mk: e.tensor_tensor(out=PT[r], in0=PT[r], in1=mk, op=ALU.mult), [B_PT[r], mkB], [B_PT[r]])
            P.op("pe", lambda e, kb=kb, r=r: e.matmul(acc, lhsT=v_lhsT(kb), rhs=PT[r], start=(kb == 0), stop=(kb == nkb - 1)),
                 [B_PT[r], vB], [accB])
        ri = acc_rot["i"] % 2
        P.op("dve", lambda e: e.reciprocal(out=rsb[ri][64:128, :], in_=acc[64:128, :]), [accB], [B_rs[ri]])
        P.op("dve", lambda e: e.tensor_tensor(out=out_ap, in0=acc[0:64, :], in1=rsb[ri][64:128, :], op=ALU.mult),
             [accB, B_rs[ri]], [outB])

    def oproj_tile(es, Wo, attnT, B_attn, hT, B_h, tag):
        wo = sbs(es, f"wo_{tag}", [128, 8, D], BF16)
        B_wo = Buf()
        load_w(wo, B_wo, Wo)
        for o in range(8):
            pb, pbB = proj_fm(wo, B_wo, 8, o * 128, 128, attnT, B_attn)
            P.op("dve", lambda e, pb=pb, o=o: e.tensor_tensor(out=hT[:, o, :], in0=pb, in1=hT[:, o, :], op=ALU.add),
                 [pbB, B_h], [B_h])

    def dsa_layer(l, es_l, strips, B_strips, cst, first):
        kT2 = sbs(es_l, "kT2", [128, S], BF16)
        kiT2 = sbs(es_l, "kiT2", [128, S], BF16)
        Vaug = sbs(es_l, "Vaug", [128, 32, 128], BF16)
        gq = sbs(es_l, "gq", [128, 2], F32)
        B_k, B_ki, B_V_, B_g = Buf(), Buf(), Buf(), Buf()
        P.op("pool", lambda e: e.memset(Vaug[:, :, 64:128], 1.0), [], [B_V_])
        for hf in range(2):
            P.dma(gq[hf * 64:(hf + 1) * 64, 0:1], a_q_norm[l].rearrange("(p o) -> p o", o=1), writes=[B_g], allow_slow_non_contiguous=True)
            P.dma(gq[hf * 64:(hf + 1) * 64, 1:2], a_k_norm[l].rearrange("(p o) -> p o", o=1), writes=[B_g], allow_slow_non_contiguous=True)
        P.op("dve", lambda e: e.tensor_scalar(out=gq[:, 0:1], in0=gq[:, 0:1], scalar1=0.125, scalar2=None, op0=ALU.mult), [B_g], [B_g])
        CW = 1.0 / (8.0 * math.sqrt(8.0))
        for j in range(NT):
            t0 = j * T
            W = (j + 1) * T
            with ExitStack() as es_t:
                hT = sbs(es_t, "hT", [128, 8, T], F32)
                B_h = Buf()
                src = xT if first else Hs
                P.dma(hT, src.rearrange("kc p t -> p kc t")[:, :, t0:t0 + T], reads=[B_Hs[j]], writes=[B_h])
                es_a = ExitStack()
                qT = sbs(es_a, "qT", [128, 8, T], BF16)
                qiT = sbs(es_a, "qiT", [128, 4, T], BF16)
                wtok = sbs(es_a, "wtok", [128, 4, 8], F32)
                attnT = sbs(es_a, "attnT", [128, 8, T], BF16)
                maskb = sbs(es_a, "maskb", [128, 4, S], BF16)
                B_q, B_qi, B_w, B_attn = Buf(), Buf(), Buf(), Buf()
                B_mb = [Buf() for _ in range(4)]
                with ExitStack() as es:
                    xn = sbs(es, "xn", [128, 8, T], BF16)
                    B_xn = Buf()
                    wi = sbs(es, "wi", [128, 8, WIC], BF16)
                    B_wi = Buf()
                    load_w(wi, B_wi, Wi_s[l])
                    rms_featmajor(es, hT, B_h, 8, D, xn, B_xn, "a")
                    sq2 = [sbs(es, f"sq2_{i}", [128, T], BF16) for i in range(2)]
                    rs2 = [sbs(es, f"rs2_{i}", [128, T], F32) for i in range(2)]
                    B_sq2 = [Buf(), Buf()]
                    B_rs2 = [Buf(), Buf()]
                    for c in range(14):
                        pb, pbB = proj_fm(wi, B_wi, 8, c * 128, 128, xn, B_xn)
                        if c < 9:
                            r = c % 2
                            P.op("act", lambda e, pb=pb, r=r: e.activation(out=sq2[r], in_=pb, func=AF.Square), [pbB], [B_sq2[r]])
                            p2, p2B = ps_short()
                            P.op("pe", lambda e, p2=p2, r=r: e.matmul(p2, lhsT=blk2b, rhs=sq2[r], start=True, stop=True),
                                 [B_sq2[r], B_const], [p2B])
                            P.op("dve", lambda e, p2=p2, r=r: e.tensor_scalar(out=rs2[r], in0=p2, scalar1=1.0 / 64, scalar2=EPS, op0=ALU.mult, op1=ALU.add),
                                 [p2B], [B_rs2[r]])
                            P.op("act", lambda e, r=r: e.activation(out=rs2[r], in_=rs2[r], func=AF.Ln), [B_rs2[r]], [B_rs2[r]])
                            P.op("act", lambda e, r=r: e.activation(out=rs2[r], in_=rs2[r], func=AF.Exp, scale=-0.5), [B_rs2[r]], [B_rs2[r]])
                            if c < 8:
                                dst, dB, gc = qT[:, c, :], B_q, gq[:, 0:1]
                            else:
                                dst, dB, gc = kT2[:, t0:t0 + T], B_k, gq[:, 1:2]
                            P.op("dve", lambda e, pb=pb, r=r, dst=dst, gc=gc: e.scalar_tensor_tensor(out=dst, in0=pb, scalar=gc, in1=rs2[r], op0=ALU.mult, op1=ALU.mult),
                                 [pbB, B_rs2[r], B_g], [dB])
                        elif c == 9:
                            P.op("act", lambda e, pb=pb: e.copy(out=kiT2[:, t0:t0 + T], in_=pb), [pbB], [B_ki])
                        else:
                            P.op("act", lambda e, pb=pb, c=c: e.copy(out=qiT[:, c - 10, :], in_=pb), [pbB], [B_qi])
                    for tb in range(4):
                        pb, pbB = ps_short()

                        def mm(e, pb=pb, tb=tb):
                            ins = None
                            for kc in range(8):
                                ins = e.matmul(pb[:, 0:72], lhsT=xn[:, kc, tb * 128:(tb + 1) * 128], rhs=wi[:, kc, 1792:1864],
                                               start=(kc == 0), stop=(kc == 7))
                            return ins
                        P.op("pe", mm, [B_xn, B_wi], [pbB])
                        P.op("dve", lambda e, pb=pb, tb=tb: e.tensor_copy(out=Vaug[:, 4 * j + tb, 0:64], in_=pb[:, 0:64]), [pbB], [B_V_])
                        P.op("dve", lambda e, pb=pb, tb=tb: e.tensor_copy(out=wtok[:, tb, :], in_=pb[:, 64:72]), [pbB], [B_w])
                    P.barrier()
                ckpt(f"proj{j}")
                with ExitStack() as es:
                    score4 = sbs(es, "score4", [128, 4, S], F32)
                    B_sc = [Buf() for _ in range(4)]
                    Rb = [sbs(es, f"Rb{i}", [128, T], BF16) for i in range(4)]
                    B_R = [Buf() for _ in range(4)]
                    Dg4 = sbs(es, "Dg4", [128, 32, 128], BF16)
                    aw4 = sbs(es, "aw4", [128, 32], F32)
                    sg4 = sbs(es, "sg4", [128, 32], F32)
                    st4 = sbs(es, "st4", [128, 8, 4], F32)
                    thr4 = sbs(es, "thr4", [128, 4], F32)
                    halves4 = sbs(es, "halves4", [128, 4, NSTEP], F32)
                    B_aw, B_Dg, B_lo, B_mid, B_nm, B_cD, B_cA, B_thr = [Buf() for _ in range(8)]
                    wt2 = wtok.rearrange("p i h -> p (i h)")
                    P.op("act", lambda e: e.activation(out=aw4, in_=wt2, func=AF.Abs, scale=CW), [B_w], [B_aw])
                    P.op("act", lambda e: e.activation(out=sg4, in_=wt2, func=AF.Sign), [B_w], [B_aw])
                    P.op("dve", lambda e: e.tensor_tensor(out=Dg4, in0=identb.unsqueeze(1).to_broadcast([128, 32, 128]),
                                                           in1=sg4.unsqueeze(2).to_broadcast([128, 32, 128]), op=ALU.mult),
                         [B_aw, B_const], [B_Dg])
                    P.op("pool", lambda e: e.memset(thr4[:, 0:2], 255.5), [], [B_thr])
                    P.op("pool", lambda e: e.memset(thr4[:, 2:4], 511.0 - W), [], [B_thr])
                    for i in range(4):
                        for kt in range(j + 1):
                            ai = acc_rot["i"] % 2
                            acc_rot["i"] += 1
                            acc, accB = banks[ai], bbufs[ai]
                            dl = []

                            def issue_d(h8, kt=kt, i=i):
                                pb, pbB = ps_short()
                                hp = (h8 % 2) * 64
                                P.op("pe", lambda e, pb=pb: e.matmul(pb, lhsT=qiT[hp:hp + 64, h8 // 2, i * 128:(i + 1) * 128],
                                                                     rhs=kiT2[hp:hp + 64, kt * T:(kt + 1) * T], start=True, stop=True),
                                     [B_qi, B_ki], [pbB])
                                dl.append((pb, pbB))
                            issue_d(0)
                            issue_d(1)
                            for h8 in range(8):
                                if h8 + 2 < 8:
                                    issue_d(h8 + 2)
                                pb, pbB = dl[h8]
                                r = h8 % 4
                                awc = aw4[:, i * 8 + h8:i * 8 + h8 + 1]
                                if h8 % 2 == 0:
                                    P.op("act", lambda e, pb=pb, r=r, awc=awc: e.activation(out=Rb[r], in_=pb, func=AF.Relu, scale=awc),
                                         [pbB, B_aw], [B_R[r]])
                                else:
                                    P.op("dve", lambda e, pb=pb, r=r, awc=awc: e.tensor_scalar(out=Rb[r], in0=pb, scalar1=awc, scalar2=0.0, op0=ALU.mult, op1=ALU.max),
                                         [pbB, B_aw], [B_R[r]])
                                P.op("pe", lambda e, r=r, h8=h8, acc=acc, i=i: e.matmul(acc, lhsT=Dg4[:, i * 8 + h8, :], rhs=Rb[r], start=(h8 == 0), stop=(h8 == 7)),
                                     [B_R[r], B_Dg], [accB])
                            P.op("act", lambda e, acc=acc, kt=kt, i=i: e.copy(out=score4[:, i, kt * T:(kt + 1) * T], in_=acc), [accB], [B_sc[i]])
                    P.op("dve", lambda e: e.tensor_reduce(out=st4[:, 0, :], in_=score4[:, :, 0:W], axis=AX.X, op=ALU.max, apply_absolute_value=True),
                         B_sc, [B_lo])
                    P.op("dve", lambda e: e.tensor_scalar(out=st4[:, 1, :], in0=st4[:, 0, :], scalar1=-1.001, scalar2=-1e-6, op0=ALU.mult, op1=ALU.add),
                         [B_lo], [B_lo])
                    P.op("dve", lambda e: e.tensor_scalar(out=st4[:, 2, :], in0=st4[:, 1, :], scalar1=-2.0, scalar2=None, op0=ALU.mult),
                         [B_lo], [B_lo])
                    P.op("dve", lambda e: e.tensor_tensor(out=halves4, in0=pw.unsqueeze(1).to_broadcast([128, 4, NSTEP]),
                                                           in1=st4[:, 2, :].unsqueeze(2).to_broadcast([128, 4, NSTEP]), op=ALU.mult),
                         [B_lo, B_const], [B_lo])
                    for i in range(4):
                        n_adm = (4 * j + i + 1) * 128
                        if n_adm - 64 < W:
                            P.op("pool", lambda e, n_adm=n_adm, i=i: e.memset(score4[0:64, i, n_adm - 64:W], -1e30), [B_lo], [B_sc[i]])
                        if n_adm < W:
                            P.op("pool", lambda e, n_adm=n_adm, i=i: e.memset(score4[64:128, i, n_adm:W], -1e30), [B_lo], [B_sc[i]])
                    for s_ in range(NSTEP):
                        hs = halves4[:, :, s_]
                        P.op("dve", lambda e, hs=hs: e.tensor_tensor(out=st4[:, 3, :], in0=st4[:, 1, :], in1=hs, op=ALU.add), [B_lo], [B_mid])
                        P.op("dve", lambda e: e.tensor_scalar(out=st4[:, 7, 2:4], in0=st4[:, 3, 2:4], scalar1=-1.0, scalar2=None, op0=ALU.mult),
                             [B_mid], [B_nm])
                        P.op("pool", lambda e: e.memset(st4[:, 4, 2:4], 0.0), [], [B_cA])
                        for i in range(2):
                            P.op("dve", lambda e, i=i: e.tensor_scalar(out=maskb[:, i, 0:W], in0=score4[:, i, 0:W], scalar1=st4[:, 3, i:i + 1], scalar2=None,
                                                                      op0=ALU.is_gt, op1=ALU.add, accum_out=st4[:, 4, i:i + 1]),
                                 [B_sc[i], B_mid], [B_mb[i], B_cD])
                        for i in range(2, 4):
                            P.op("act", lambda e, i=i: e.activation(out=maskb[:, i, 0:W], in_=score4[:, i, 0:W], func=AF.Sign, bias=st4[:, 7, i:i + 1],
                                                                   scale=1.0, accum_out=st4[:, 4, i:i + 1]),
                                 [B_sc[i], B_nm], [B_mb[i], B_cA])
                        P.op("dve", lambda e: e.tensor_tensor(out=st4[:, 5, :], in0=st4[:, 4, :], in1=thr4, op=ALU.is_ge), [B_cD, B_cA, B_thr], [B_lo])
                        P.op("dve", lambda e, hs=hs: e.tensor_tensor(out=st4[:, 6, :], in0=st4[:, 5, :], in1=hs, op=ALU.mult), [B_lo], [B_lo])
                        P.op("dve", lambda e: e.tensor_tensor(out=st4[:, 1, :], in0=st4[:, 1, :], in1=st4[:, 6, :], op=ALU.add), [B_lo], [B_lo])
                    for i in range(4):
                        P.op("dve", lambda e, i=i: e.tensor_scalar(out=maskb[:, i, 0:W], in0=score4[:, i, 0:W], scalar1=st4[:, 1, i:i + 1], scalar2=NEGM,
                                                                  op0=ALU.is_le, op1=ALU.mult),
                             [B_sc[i], B_lo], [B_mb[i]])
                    P.barrier()
                ckpt(f"idx{j}")
                with ExitStack() as es:
                    nkb = 4 * j + 4
                    maskT = sbs(es, "maskT", [128, 32, T], BF16)
                    B_mT = [Buf() for _ in range(32)]
                    PT = [sbs(es, f"PT{i}", [128, T], BF16) for i in range(4)]
                    B_PT = [Buf() for _ in range(4)]
                    rsb = [sbs(es, f"rsb{i}", [128, T], F32) for i in range(2)]
                    B_rs = [Buf(), Buf()]
                    for kb in range(nkb):
                        pt, ptB = ps_tr()

                        def tr(e, pt=pt, kb=kb):
                            ins = None
                            for i in range(4):
                                ins = e.transpose(pt[:, i * 128:(i + 1) * 128], maskb[:, i, kb * 128:(kb + 1) * 128], identb)
                            return ins
                        P.op("pe", tr, B_mb + [B_const], [ptB])
                        import os
                        if os.environ.get("KDBG") == "nocopy":
                            continue
                        eng = "act" if kb % 2 == 0 else "dve"
                        if eng == "act":
                            P.op("act", lambda e, pt=pt, kb=kb: e.copy(out=maskT[:, kb, :], in_=pt), [ptB], [B_mT[kb]])
                        else:
                            P.op("dve", lambda e, pt=pt, kb=kb: e.tensor_copy(out=maskT[:, kb, :], in_=pt), [ptB], [B_mT[kb]])
                    ckpt(f"tr{j}")
                    for h in range(16):
                        ckpt(f"head{j}_{h}")
                        hp = (h % 2) * 64
                        c = h // 2

                        def s_mm(e, pb, kb, hp=hp, c=c, h=h):
                            near = kb >= 4 * j - 1
                            e.matmul(pb, lhsT=kT2[hp:hp + 64, kb * 128:(kb + 1) * 128], rhs=qT[hp:hp + 64, c, :], start=True, stop=False)
                            ins = e.matmul(pb, lhsT=identb, rhs=maskT[:, kb, :], start=False, stop=(not near))
                            if near:
                                off = (3 - (kb - 4 * j)) * 128
                                ins = e.matmul(pb, lhsT=identb, rhs=strips[:, h, off:off + T], start=False, stop=True)
                            return ins

                        def exp_args(kb, h=h):
                            if kb >= 4 * j - 1:
                                return zeros[:, 0:1], 1.0
                            return cst[:, h:h + 1], 1.0

                        attn_head(nkb, s_mm, exp_args, lambda kb: Vaug[:, kb, :], B_V_, PT, B_PT, rsb, B_rs,
                                  attnT[hp:hp + 64, c, :], B_attn,
                                  lambda kb: [B_k, B_q, B_mT[kb], B_strips, B_const])
                    P.barrier()
                ckpt(f"attn{j}")
                with ExitStack() as es:
                    oproj_tile(es, Wao_s[l], attnT, B_attn, hT, B_h, "a")
                    P.barrier()
                es_a.close()
                ckpt(f"oproj{j}")
                dstH = outT if (l == n_layers - 1) else Hs
                mlp_tile(l, hT, B_h, store=lambda: P.dma(dstH.rearrange("kc p t -> p kc t")[:, :, t0:t0 + T], hT, reads=[B_h], writes=[B_Hs[j]]))
                ckpt(f"mlp{j}")

    def qk_tmps(es, tag):
        sq = sbs(es, f"qk_sq_{tag}", [128, 16, 96], F32)
        ss = sbs(es, f"qk_ss_{tag}", [128, 16], F32)
        t1 = sbs(es, f"qk_t1_{tag}", [128, 16, 32], F32)
        t2 = sbs(es, f"qk_t2_{tag}", [128, 16, 32], F32)
        return (sq, ss, t1, t2, Buf(), Buf(), Buf(), Buf())

    def qk_finish(tm, full, B_full, gain_b, cs, sn, B_cs, outb, B_out):
        sq, ss, t1, t2, Bs, Bss, Bt1, Bt2 = tm
        P.op("act", lambda e: e.activation(out=sq, in_=full, func=AF.Square), [B_full], [Bs])
        P.op("dve", lambda e: e.tensor_reduce(out=ss, in_=sq, axis=AX.X, op=ALU.add), [Bs], [Bss])
        P.op("dve", lambda e: e.tensor_scalar(out=ss, in0=ss, scalar1=1.0 / 96, scalar2=EPS, op0=ALU.mult, op1=ALU.add), [Bss], [Bss])
        P.op("act", lambda e: e.activation(out=ss, in_=ss, func=AF.Ln), [Bss], [Bss])
        P.op("act", lambda e: e.activation(out=ss, in_=ss, func=AF.Exp, scale=-0.5), [Bss], [Bss])
        P.op("dve", lambda e: e.tensor_tensor(out=full, in0=full, in1=ss.unsqueeze(2).to_broadcast([128, 16, 96]), op=ALU.mult),
             [B_full, Bss], [B_full])
        P.op("dve", lambda e: e.tensor_tensor(out=full, in0=full, in1=gain_b.unsqueeze(1).to_broadcast([128, 16, 96]), op=ALU.mult),
             [B_full, B_const], [B_full])
        P.op("pool", lambda e: e.tensor_copy(out=outb[:, :, 0:64], in_=full[:, :, 0:64]), [B_full], [B_out])
        P.op("dve", lambda e: e.tensor_tensor(out=t1, in0=full[:, :, 64:96], in1=cs.unsqueeze(1).to_broadcast([128, 16, 32]), op=ALU.mult),
             [B_full, B_cs], [Bt1])
        P.op("dve", lambda e: e.tensor_tensor(out=t2[:, :, 0:16], in0=full[:, :, 80:96], in1=sn[:, 0:16].unsqueeze(1).to_broadcast([128, 16, 16]), op=ALU.mult),
             [B_full, B_cs], [Bt2])
        P.op("dve", lambda e: e.tensor_tensor(out=t2[:, :, 16:32], in0=full[:, :, 64:80], in1=sn[:, 16:32].unsqueeze(1).to_broadcast([128, 16, 16]), op=ALU.mult),
             [B_full, B_cs, Bt2], [Bt2])
        P.op("dve", lambda e: e.tensor_tensor(out=outb[:, :, 64:96], in0=t1, in1=t2, op=ALU.add), [Bt1, Bt2, B_out], [B_out])

    def lat_norm(es, src_ps_list, KC, Dn, dst, dstB, tag):
        raw = sbs(es, f"ln_raw_{tag}", [128, KC, T], F32)
        Braw = Buf()
        for kc, (pb, pbB) in enumerate(src_ps_list):
            P.op("act", lambda e, pb=pb, kc=kc: e.copy(out=raw[:, kc, :], in_=pb), [pbB], [Braw])
        rms_featmajor(es, raw, Braw, KC, Dn, dst, dstB, tag)

    def mla_layer(lb, layer, es_l, gen_kv, gk_b, gq_b):
        for j in range(NT):
            t0 = j * T
            with ExitStack() as es_t:
                hT = sbs(es_t, "hT", [128, 8, T], F32)
                B_h = Buf()
                P.dma(hT, Hs.rearrange("kc p t -> p kc t")[:, :, t0:t0 + T], reads=[B_Hs[j]], writes=[B_h])
                qT = sbs(es_t, "b_qT", [128, 16, T], BF16)
                B_qT = Buf()
                attnT = sbs(es_t, "attnT", [128, 8, T], BF16)
                B_attn = Buf()
                cs = sbs(es_t, "cs", [128, 4, 32], F32)
                sn = sbs(es_t, "sn", [128, 4, 32], F32)
                B_cs = Buf()
                P.dma(cs, c_cos[t0:t0 + T, :].rearrange("(tb p) d -> p tb d", p=128), writes=[B_cs])
                P.dma(sn, c_sinm[t0:t0 + T, :].rearrange("(tb p) d -> p tb d", p=128), writes=[B_cs])
                if gen_kv:
                    with ExitStack() as es:
                        xn = sbs(es, "xn", [128, 8, T], BF16)
                        B_xn = Buf()
                        wd = sbs(es, "wdkv", [128, 8, 288], BF16)
                        wu = sbs(es, "wukv", [128, 2, 2048], BF16)
                        B_wd, B_wu = Buf(), Buf()
                        load_w(wd, B_wd, Wdkv_s)
                        load_w(wu, B_wu, Wukv_s)
                        rms_featmajor(es, hT, B_h, 8, D, xn, B_xn, "kv")
                        lat = sbs(es, "lat", [128, 2, T], BF16)
                        B_lat = Buf()
                        pl = [proj_fm(wd, B_wd, 8, c * 128, 128, xn, B_xn) for c in range(2)]
                        lat_norm(es, pl, 2, 256, lat, B_lat, "lat")
                        KTst = sbs(es, "KTst", [128, 16, T], BF16)
                        B_KTst = Buf()
                        kfull = [sbs(es, f"kfull{i}", [128, 16, 96], F32) for i in range(2)]
                        kb16 = [sbs(es, f"kb16{i}", [128, 16, 96], BF16) for i in range(2)]
                        vtok = [sbs(es, f"vtok{i}", [128, 16, 64], BF16) for i in range(2)]
                        B_kf = [Buf(), Buf()]
                        B_kb = [Buf(), Buf()]
                        B_vt = [Buf(), Buf()]
                        ktm = qk_tmps(es, "k")
                        for tb in range(4):
                            r = tb % 2
                            pr, prB = ps_short()

                            def mmr(e, pr=pr, tb=tb):
                                ins = None
                                for kc in range(8):
                                    ins = e.matmul(pr[:, 0:32], lhsT=xn[:, kc, tb * 128:(tb + 1) * 128], rhs=wd[:, kc, 256:288],
                                                   start=(kc == 0), stop=(kc == 7))
                                return ins
                            P.op("pe", mmr, [B_xn, B_wd], [prB])
                            P.op("dve", lambda e, pr=pr, r=r: e.tensor_copy(out=kfull[r][:, :, 64:96], in_=pr[:, 0:32].unsqueeze(1).to_broadcast([128, 16, 32])),
                                 [prB], [B_kf[r]])
                            for cg in range(4):
                                pk, pkB = ps_short()

                                def mmk(e, pk=pk, tb=tb, cg=cg):
                                    ins = None
                                    for c2 in range(2):
                                        ins = e.matmul(pk, lhsT=lat[:, c2, tb * 128:(tb + 1) * 128], rhs=wu[:, c2, cg * 512:(cg + 1) * 512],
                                                       start=(c2 == 0), stop=(c2 == 1))
                                    return ins
                                P.op("pe", mmk, [B_lat, B_wu], [pkB])
                                pk3 = pk.rearrange("p (h d) -> p h d", h=4)
                                if cg % 2 == 0:
                                    P.op("act", lambda e, pk3=pk3, r=r, cg=cg: e.copy(out=kfull[r][:, 4 * cg:4 * cg + 4, 0:64], in_=pk3[:, :, 0:64]),
                                         [pkB], [B_kf[r]])
                                    P.op("act", lambda e, pk3=pk3, r=r, cg=cg: e.copy(out=vtok[r][:, 4 * cg:4 * cg + 4, :], in_=pk3[:, :, 64:128]),
                                         [pkB], [B_vt[r]])
                                else:
                                    P.op("dve", lambda e, pk3=pk3, r=r, cg=cg: e.tensor_copy(out=kfull[r][:, 4 * cg:4 * cg + 4, 0:64], in_=pk3[:, :, 0:64]),
                                         [pkB], [B_kf[r]])
                                    P.op("dve", lambda e, pk3=pk3, r=r, cg=cg: e.tensor_copy(out=vtok[r][:, 4 * cg:4 * cg + 4, :], in_=pk3[:, :, 64:128]),
                                         [pkB], [B_vt[r]])
                            P.dma(V_s[:, :, 4 * j + tb, :].rearrange("h p d -> p h d"), vtok[r], reads=[B_vt[r]], writes=[B_V[j]])
                            qk_finish(ktm, kfull[r], B_kf[r], gk_b, cs[:, tb, :], sn[:, tb, :], B_cs, kb16[r], B_kb[r])
                            for hg in range(4):
                                pt, ptB = ps_tr()

                                def tr(e, pt=pt, r=r, hg=hg):
                                    ins = None
                                    for hh in range(4):
                                        ins = e.transpose(pt[0:96, hh * 128:(hh + 1) * 128], kb16[r][:, 4 * hg + hh, :], identb)
                                    return ins
                                P.op("pe", tr, [B_kb[r], B_const], [ptB])
                                pt3 = pt.rearrange("p (h t) -> p h t", h=4)
                                P.op("act", lambda e, pt3=pt3, hg=hg, tb=tb: e.copy(out=KTst[0:96, 4 * hg:4 * hg + 4, tb * 128:(tb + 1) * 128], in_=pt3[0:96, :, :]),
                                     [ptB], [B_KTst])
                        P.dma(KT_s[:, :, t0:t0 + T].rearrange("h d t -> d h t"), KTst[0:96, :, :], reads=[B_KTst], writes=[B_KT[j]])
                        P.barrier()
                ckpt(f"kv{j}")
                with ExitStack() as es:
                    xn = sbs(es, "xn", [128, 8, T], BF16)
                    B_xn = Buf()
                    wd = sbs(es, "wdq", [128, 8, 384], BF16)
                    wu = sbs(es, "wuq", [128, 3, 1536], BF16)
                    B_wd, B_wu = Buf(), Buf()
                    load_w(wd, B_wd, Wdq_s[lb])
                    load_w(wu, B_wu, Wuq_s[lb])
                    rms_featmajor(es, hT, B_h, 8, D, xn, B_xn, "q")
                    cq = sbs(es, "cq", [128, 3, T], BF16)
                    B_cq = Buf()
                    pl = [proj_fm(wd, B_wd, 8, c * 128, 128, xn, B_xn) for c in range(3)]
                    lat_norm(es, pl, 3, 384, cq, B_cq, "cq")
                    qfull = [sbs(es, f"qfull{i}", [128, 16, 96], F32) for i in range(2)]
                    qb16 = [sbs(es, f"qb16{i}", [128, 16, 96], BF16) for i in range(2)]
                    B_qf = [Buf(), Buf()]
                    B_qb = [Buf(), Buf()]
                    qtm = qk_tmps(es, "q")
                    for tb in range(4):
                        r = tb % 2
                        for cg in range(3):
                            pq, pqB = ps_short()

                            def mmq(e, pq=pq, tb=tb, cg=cg):
                                ins = None
                                for c3 in range(3):
                                    ins = e.matmul(pq, lhsT=cq[:, c3, tb * 128:(tb + 1) * 128], rhs=wu[:, c3, cg * 512:(cg + 1) * 512],
                                                   start=(c3 == 0), stop=(c3 == 2))
                                return ins
                            P.op("pe", mmq, [B_cq, B_wu], [pqB])
                            qf2 = qfull[r].rearrange("p h d -> p (h d)")
                            P.op("act", lambda e, pq=pq, qf2=qf2, cg=cg: e.copy(out=qf2[:, cg * 512:(cg + 1) * 512], in_=pq), [pqB], [B_qf[r]])
                        qk_finish(qtm, qfull[r], B_qf[r], gq_b, cs[:, tb, :], sn[:, tb, :], B_cs, qb16[r], B_qb[r])
                        for hg in range(4):
                            pt, ptB = ps_tr()

                            def tr(e, pt=pt, r=r, hg=hg):
                                ins = None
                                for hh in range(4):
                                    ins = e.transpose(pt[0:96, hh * 128:(hh + 1) * 128], qb16[r][:, 4 * hg + hh, :], identb)
                                return ins
                            P.op("pe", tr, [B_qb[r], B_const], [ptB])
                            pt3 = pt.rearrange("p (h t) -> p h t", h=4)
                            P.op("dve", lambda e, pt3=pt3, hg=hg, tb=tb: e.tensor_copy(out=qT[0:96, 4 * hg:4 * hg + 4, tb * 128:(tb + 1) * 128], in_=pt3[0:96, :, :]),
                                 [ptB], [B_qT])
                    P.barrier()
                ckpt(f"q{j}")
                with ExitStack() as es:
                    nkb = 4 * j + 4
                    nk = nkb * 128
                    KTh = [sbs(es, f"KTh{i}", [128, S], BF16) for i in range(2)]
                    Vh = [sbs(es, f"Vh{i}", [128, 32, 128], BF16) for i in range(2)]
                    B_KTh = [Buf(), Buf()]
                    B_Vh = [Buf(), Buf()]
                    PT = [sbs(es, f"PT{i}", [128, T], BF16) for i in range(4)]
                    B_PT = [Buf() for _ in range(4)]
                    rsb = [sbs(es, f"rsb{i}", [128, T], F32) for i in range(2)]
                    B_rs = [Buf(), Buf()]
                    for i in range(2):
                        P.op("pool", lambda e, i=i: e.memset(Vh[i][:, :, 64:128], 1.0), [], [B_Vh[i]])

                    def load_head(h):
                        r = h % 2
                        P.dma(KTh[r][0:96, 0:nk], KT_s[h, :, 0:nk], reads=B_KT[0:j + 1], writes=[B_KTh[r]])
                        P.dma(Vh[r][:, 0:nkb, 0:64], V_s[h, :, 0:nkb, :], reads=B_V[0:j + 1], writes=[B_Vh[r]])
                    load_head(0)
                    SC = 96 ** -0.5
                    for h in range(16):
                        if h + 1 < 16:
                            load_head(h + 1)
                        r = h % 2
                        hp = (h % 2) * 64
                        c = h // 2

                        def s_mm(e, pb, kb, h=h, r=r):
                            near = kb >= 4 * j
                            ins = e.matmul(pb, lhsT=KTh[r][0:96, kb * 128:(kb + 1) * 128], rhs=qT[0:96, h, :], start=True, stop=(not near))
                            if near:
                                off = (3 - (kb - 4 * j)) * 128
                                ins = e.matmul(pb, lhsT=identb, rhs=mstrip[:, off:off + T], start=False, stop=True)
                            return ins

                        attn_head(nkb, s_mm, lambda kb: (zeros[:, 0:1], SC), lambda kb, r=r: Vh[r][:, kb, :], B_Vh[r], PT, B_PT, rsb, B_rs,
                                  attnT[hp:hp + 64, c, :], B_attn,
                                  lambda kb, r=r: [B_KTh[r], B_qT, B_const])
                    P.barrier()
                ckpt(f"battn{j}")
                with ExitStack() as es:
                    oproj_tile(es, Wbo_s[lb], attnT, B_attn, hT, B_h, "b")
                    P.barrier()
                ckpt(f"boproj{j}")
                last = (layer == n_layers - 1)
                dstH = outT if last else Hs
                mlp_tile(layer, hT, B_h, store=lambda: P.dma(dstH.rearrange("kc p t -> p kc t")[:, :, t0:t0 + T], hT, reads=[B_h], writes=[B_Hs[j]]))

    try:
        nA = min(2, n_layers)
        with ExitStack() as es_dsa:
            strips = sbs(es_dsa, "strips", [128, 16, 1024], BF16)
            cst = sbs(es_dsa, "cst", [128, 16], F32)
            B_strips = Buf()
            with ExitStack() as es:
                rb = sbs(es, "rb", [32, 16], F32)
                oh = sbs(es, "oh", [32, 1152], F32)
                gt = sbs(es, "gt", [16, 1152], F32)
                wn = sbs(es, "wn", [128, 1024], F32)
                mst = sbs(es, "mst", [128, 1024], F32)
                Brb, Bgt, Bwn, Bmst = Buf(), Buf(), Buf(), Buf()
                P.dma(rb, rel_bias, writes=[Brb])
                P.dma(oh, c_oh, writes=[Brb])
                P.dma(mst, c_mstrip, writes=[Bmst])
                for cch in range(3):
                    pb, pbB = ps_short()
                    P.op("pe", lambda e, pb=pb, cch=cch: e.matmul(pb[0:16, 0:384], lhsT=rb, rhs=oh[:, cch * 384:(cch + 1) * 384], start=True, stop=True),
                         [Brb], [pbB])
                    P.op("dve", lambda e, pb=pb, cch=cch: e.tensor_copy(out=gt[:, cch * 384:(cch + 1) * 384], in_=pb[0:16, 0:384]), [pbB], [Bgt])
                P.dma(G_s, gt, reads=[Bgt], writes=[B_G])
                for h in range(16):
                    wap = bass.AP(G_s.tensor, h * 1152, [[1, 128], [1, 1024]])
                    P.dma(wn, wap, reads=[B_G], writes=[Bwn])
                    wrev = bass.AP(wn.tensor, wn.offset + 1023, [list(wn.ap[0]), [-1, 1024]])
                    P.op("dve", lambda e, wrev=wrev, h=h: e.tensor_tensor(out=strips[:, h, :], in0=wrev, in1=mst, op=ALU.add),
                         [Bwn, Bmst], [B_strips])
                    P.op("dve", lambda e, h=h: e.tensor_copy(out=cst[:, h:h + 1], in_=wn[:, 0:1]), [Bwn], [B_strips])
                P.barrier()
            for l in range(nA):
                with ExitStack() as es_l:
                    ckpt("strips")
                    ckpt("consts")
                    dsa_layer(l, es_l, strips, B_strips, cst, first=(l == 0))
                    P.barrier()
            P.barrier()
        if n_layers > 2:
            gk_b = sbp("gk_b", [128, 96], F32)
            gq_bs = [sbp(f"gq_b{i}", [128, 96], F32) for i in range(2)]
            P.dma(gk_b, bass.AP(k_norm.tensor, 0, [[0, 128], [1, 96]]), writes=[B_const])
            for i in range(2):
                P.dma(gq_bs[i], bass.AP(b_q_norm.tensor, i * 96, [[0, 128], [1, 96]]), writes=[B_const])
            for lb in range(n_layers - 2):
                with ExitStack() as es_l:
                    mla_layer(lb, 2 + lb, es_l, gen_kv=(lb == 0), gk_b=gk_b, gq_b=gq_bs[lb])
                    P.barrier()
    except _Stop:
        pass
    P.barrier()
    P.emit()
    return nc


_CACHE = {}


def kernel(**inputs):
    n_layers = 4
    if "nc" not in _CACHE:
        _CACHE["nc"] = build(n_layers)
        _CACHE["consts"] = _consts()
    nc = _CACHE["nc"]
    cs = _CACHE["consts"]
    x = np.asarray(inputs["x"], dtype=np.float32)
    shared = {k: np.ascontiguousarray(np.asarray(v, dtype=np.float32)) for k, v in inputs.items() if k != "x"}
    shared.update({"c_ident": cs["ident"], "c_blk2": cs["blk2"], "c_oh": cs["oh"], "c_mstrip": cs["mstrip"],
                   "c_cos": cs["cos"], "c_sinm": cs["sinm"], "c_pw": cs["pw"]})
    in_maps = []
    for b in range(8):
        m = dict(shared)
        m["xT"] = np.ascontiguousarray(x[b].T).reshape(8, 128, S)
        in_maps.append(m)
    res = run_bass_kernel_spmd(nc, in_maps, core_ids=list(range(8)))
    out = np.stack([np.ascontiguousarray(r["outT"].reshape(D, S).T) for r in res.results], 0)
    return out.astype(np.float32)
```

```python
import math
from contextlib import ExitStack
import numpy as np
import ml_dtypes
import concourse.bass as bass
import concourse.mybir as mybir
from concourse.bass_utils import run_bass_kernel_spmd

F32 = mybir.dt.float32
BF16 = mybir.dt.bfloat16
ALU = mybir.AluOpType
AF = mybir.ActivationFunctionType
AX = mybir.AxisListType

S = 4096
D = 1024
T = 512
NT = S // T
DFF = 4096
EPS = 1e-6
NEGM = -30000.0
NSTEP = 18
WIC = 1864


class Buf:
    __slots__ = ("name", "w", "r")

    def __init__(self, name=""):
        self.name = name
        self.w = None
        self.r = {}


class Op:
    __slots__ = ("eng", "fn", "deps", "sig", "ticket", "dma")

    def __init__(self, eng, fn, dma=False):
        self.eng = eng
        self.fn = fn
        self.deps = []
        self.sig = False
        self.ticket = None
        self.dma = dma


import types


def _freeze(fn, memo=None):
    if memo is None:
        memo = {}
    if not isinstance(fn, types.FunctionType) or fn.__closure__ is None:
        return fn
    if id(fn) in memo:
        return memo[id(fn)]
    cells = []
    for c in fn.__closure__:
        try:
            v = c.cell_contents
        except ValueError:
            cells.append(c)
            continue
        if isinstance(v, types.FunctionType):
            v = _freeze(v, memo)
        cells.append(types.CellType(v))
    nf = types.FunctionType(fn.__code__, fn.__globals__, fn.__name__, fn.__defaults__, tuple(cells))
    nf.__kwdefaults__ = fn.__kwdefaults__
    memo[id(fn)] = nf
    return nf


class Prog:
    def __init__(self, nc, n_dma_sems=40, epoch=30000):
        self.nc = nc
        self.ops = []
        self.last = {}
        self.dma_live = []
        self.n_dma_sems = n_dma_sems
        self.epoch = epoch
        self.engs = {"pe": nc.tensor, "act": nc.scalar, "dve": nc.vector,
                     "pool": nc.gpsimd, "sp": nc.sync}

    def op(self, eng, fn, reads=(), writes=(), dma=False):
        o = Op(eng, _freeze(fn), dma)
        deps = {}
        for b in reads:
            if b.w is not None:
                deps[id(b.w)] = b.w
        for b in writes:
            if b.w is not None:
                if not (eng == "pe" and b.w.eng == "pe" and not b.w.dma):
                    deps[id(b.w)] = b.w
            for re_, ro in b.r.items():
                if re_ == eng and not dma and not ro.dma:
                    continue
                deps[id(ro)] = ro
        for d in deps.values():
            d.sig = True
        o.deps = list(deps.values())
        key = eng if not dma else ("dma", id(o))
        for b in reads:
            b.r[key] = o
        for b in writes:
            b.w = o
            b.r = {}
        self.ops.append(o)
        if dma:
            self.dma_live.append(o)
        else:
            self.last[eng] = o
        return o

    def dma(self, out_ap, in_ap, reads=(), writes=(), q="sp", **kw):
        def fn(e):
            return e.dma_start(out=out_ap, in_=in_ap, **kw)
        return self.op(q, fn, reads, writes, dma=True)

    def barrier(self):
        lasts = list(self.last.values())
        dmas = list(self.dma_live)
        for d in lasts + dmas:
            d.sig = True
        for e in self.engs:
            o = Op(e, None)
            o.deps = [d for d in lasts + dmas if not (d.eng == e and not d.dma)]
            self.ops.append(o)
        self.dma_live = []

    def emit(self):
        nc = self.nc
        engsem = {}
        engcnt = {}
        waited = {e: {} for e in self.engs}
        npool = {"sp": self.n_dma_sems - 8, "pool": 8}
        dsems_q = {q: [nc.alloc_semaphore(f"dq_{q}{i}") for i in range(n)] for q, n in npool.items()}
        dval_q = {q: [0] * n for q, n in npool.items()}
        dnext_q = {q: 0 for q in npool}
        nsem = 0
        for o in self.ops:
            e = self.engs[o.eng]
            w = waited[o.eng]
            for d in o.deps:
                s, v = d.ticket
                if w.get(id(s), 0) < v:
                    e.wait_ge(s, v)
                    w[id(s)] = v
            if o.fn is None:
                continue
            if o.dma:
                dsems, dval = dsems_q[o.eng], dval_q[o.eng]
                i = dnext_q[o.eng]
                dnext_q[o.eng] = (i + 1) % len(dsems)
                s = dsems[i]
                if dval[i] > 0 and w.get(id(s), 0) < dval[i]:
                    e.wait_ge(s, dval[i])
                    w[id(s)] = dval[i]
                ins = o.fn(e)
                dval[i] += 16
                ins.then_inc(s, 16)
                o.ticket = (s, dval[i])
            else:
                ins = o.fn(e)
                if o.sig:
                    if o.eng not in engsem or engcnt[o.eng] >= self.epoch:
                        engsem[o.eng] = nc.alloc_semaphore(f"e_{o.eng}_{nsem}")
                        nsem += 1
                        engcnt[o.eng] = 0
                    engcnt[o.eng] += 1
                    ins.then_inc(engsem[o.eng], 1)
                    o.ticket = (engsem[o.eng], engcnt[o.eng])


def _t5_bucket(rel):
    import jax
    import jax.numpy as jnp
    with jax.default_device(jax.devices("cpu")[0]):
        return _t5_bucket_impl(np.asarray(rel), jnp)


def _t5_bucket_impl(rel, jnp):
    rel = jnp.asarray(rel, dtype=jnp.int32)
    nb = 16
    max_exact = 8
    ret = jnp.where(rel > 0, nb, 0)
    n = jnp.abs(rel)
    nf = jnp.maximum(n, 1).astype(jnp.float32)
    large = max_exact + (jnp.log(nf / max_exact) / math.log(128 / max_exact)
                         * (nb - max_exact)).astype(jnp.int32)
    large = jnp.minimum(large, nb - 1)
    return np.asarray(ret + jnp.where(n < max_exact, n, large))


def _consts():
    c = {}
    c["ident"] = np.eye(128, dtype=np.float32)
    blk = np.zeros((128, 128), np.float32)
    blk[:64, :64] = 1.0
    blk[64:, 64:] = 1.0
    c["blk2"] = blk
    j = np.arange(1152)
    bk = _t5_bucket(j - 639)
    oh = np.zeros((32, 1152), np.float32)
    oh[bk, j] = 1.0
    c["oh"] = oh
    k = np.arange(128)[:, None]
    x = np.arange(1024)[None, :]
    s = x // 128
    qp = x % 128
    kpos = 128 * (3 - s) + k
    adm = np.floor_divide(kpos, 64) <= (qp // 64)
    c["mstrip"] = np.where(adm, 0.0, NEGM).astype(np.float32)
    pos = np.arange(S, dtype=np.float32)
    inv = (1.0 / (10000.0 ** (np.arange(0, 32, 2, dtype=np.float32) / 32))).astype(np.float32)
    ang = pos[:, None] * inv[None, :]
    emb = np.concatenate([ang, ang], -1)
    cos = np.cos(emb).astype(np.float32)
    sin = np.sin(emb).astype(np.float32)
    sinm = sin.copy()
    sinm[:, :16] = -sinm[:, :16]
    c["cos"] = cos
    c["sinm"] = sinm
    c["pw"] = np.tile((2.0 ** -(np.arange(NSTEP) + 1.0)).astype(np.float32)[None], (128, 1))
    return c


class _Stop(Exception):
    pass


def build(n_layers=4, stop=None):
    nc = bass.Bass("TRN2", target_bir_lowering=False)
    P = Prog(nc)

    def ckpt(name):
        if stop is not None and name == stop:
            raise _Stop()

    def din(name, shape, dt=F32):
        return nc.dram_tensor(name, list(shape), dt, kind="ExternalInput").ap()

    def dscr(name, shape, dt):
        return nc.dram_tensor(name, list(shape), dt).ap()

    xT = din("xT", [8, 128, S])
    rel_bias = din("rel_bias", [32, 16])
    a_attn_norm = din("a_attn_norm", [2, D])
    a_w_in = din("a_w_in", [2, D, 1736])
    a_q_norm = din("a_q_norm", [2, 64])
    a_k_norm = din("a_k_norm", [2, 64])
    a_w_o = din("a_w_o", [2, D, D])
    kv_norm = din("kv_norm", [D])
    w_dkv = din("w_dkv", [D, 288])
    kv_lora_norm = din("kv_lora_norm", [256])
    w_ukv = din("w_ukv", [256, 2048])
    k_norm = din("k_norm", [96])
    b_attn_norm = din("b_attn_norm", [2, D])
    b_w_dq = din("b_w_dq", [2, D, 384])
    b_q_lora_norm = din("b_q_lora_norm", [2, 384])
    b_w_uq = din("b_w_uq", [2, 384, 1536])
    b_q_norm = din("b_q_norm", [2, 96])
    b_w_o = din("b_w_o", [2, D, D])
    mlp_norm = din("mlp_norm", [4, D])
    mlp_w_up = din("mlp_w_up", [4, D, DFF])
    mlp_w_down = din("mlp_w_down", [4, DFF, D])
    c_ident = din("c_ident", [128, 128])
    c_blk2 = din("c_blk2", [128, 128])
    c_oh = din("c_oh", [32, 1152])
    c_mstrip = din("c_mstrip", [128, 1024])
    c_cos = din("c_cos", [S, 32])
    c_sinm = din("c_sinm", [S, 32])
    c_pw = din("c_pw", [128, NSTEP])
    outT = nc.dram_tensor("outT", [8, 128, S], F32, kind="ExternalOutput").ap()

    Hs = dscr("Hs", [8, 128, S], F32)
    Wi_s = [dscr(f"Wi{l}", [128, 8, WIC], BF16) for l in range(2)]
    Wao_s = [dscr(f"Wao{l}", [128, 8, D], BF16) for l in range(2)]
    Wbo_s = [dscr(f"Wbo{l}", [128, 8, D], BF16) for l in range(2)]
    Wup_s = [dscr(f"Wup{l}", [128, 8, DFF], BF16) for l in range(4)]
    Wdn_s = [dscr(f"Wdn{l}", [128, 32, D], BF16) for l in range(4)]
    Wdkv_s = dscr("Wdkv", [128, 8, 288], BF16)
    Wukv_s = dscr("Wukv", [128, 2, 2048], BF16)
    Wdq_s = [dscr(f"Wdq{l}", [128, 8, 384], BF16) for l in range(2)]
    Wuq_s = [dscr(f"Wuq{l}", [128, 3, 1536], BF16) for l in range(2)]
    G_s = dscr("Gtab", [16, 1152], F32)
    KT_s = dscr("KTs", [16, 96, S], BF16)
    V_s = dscr("Vs", [16, 128, 32, 64], BF16)
    B_Hs = [Buf() for _ in range(NT)]
    B_KT = [Buf() for _ in range(NT)]
    B_V = [Buf() for _ in range(NT)]
    B_G = Buf()

    def sbp(name, shape, dt):
        return nc.alloc_sbuf_tensor(name, list(shape), dt).ap()

    uid = {"i": 0}

    def sbs(es, name, shape, dt):
        uid["i"] += 1
        return es.enter_context(nc.sbuf_tensor(f"{name}_u{uid['i']}", list(shape), dt)).ap()

    banks = [nc.alloc_psum_tensor(f"pb{i}", [128, 512], F32).ap() for i in range(6)]
    bbufs = [Buf(f"pb{i}") for i in range(6)]
    psb0 = nc.alloc_psum_tensor("psb0", [128, 1024], BF16).ap()
    psb1 = nc.alloc_psum_tensor("psb1", [128, 1024], BF16).ap()
    psb_h = [psb0[:, 0:512], psb1[:, 0:512]]
    psb_b = [Buf(), Buf()]
    rot = {"i": 0, "t": 0}

    def ps_short():
        i = 2 + rot["i"] % 4
        rot["i"] += 1
        return banks[i], bbufs[i]

    def ps_tr():
        i = rot["t"] % 2
        rot["t"] += 1
        return psb_h[i], psb_b[i]

    ident = sbp("ident", [128, 128], F32)
    identb = sbp("identb", [128, 128], BF16)
    onesb = sbp("onesb", [128, 128], BF16)
    blk2b = sbp("blk2b", [128, 128], BF16)
    mstrip = sbp("mstrip", [128, 1024], BF16)
    pw = sbp("pw", [128, NSTEP], F32)
    zeros = sbp("zeros", [128, 1], F32)
    epsb = sbp("epsb", [128, 1], F32)
    B_const = Buf("const")
    with ExitStack() as es:
        t0 = sbs(es, "c_t0", [128, 1024], F32)
        bt = Buf()
        P.dma(ident, c_ident, writes=[B_const])
        P.op("dve", lambda e: e.tensor_copy(out=identb, in_=ident), [B_const], [B_const])
        P.dma(t0[:, 0:128], c_blk2, writes=[bt])
        P.op("dve", lambda e: e.tensor_copy(out=blk2b, in_=t0[:, 0:128]), [bt], [B_const, bt])
        P.dma(t0, c_mstrip, reads=[], writes=[bt])
        P.op("dve", lambda e: e.tensor_copy(out=mstrip, in_=t0), [bt], [B_const, bt])
        P.dma(pw, c_pw, writes=[B_const])
        P.op("dve", lambda e: e.memset(onesb, 1.0), [], [B_const])
        P.op("dve", lambda e: e.memset(zeros, 0.0), [], [B_const])
        P.op("dve", lambda e: e.memset(epsb, EPS), [], [B_const])
        P.barrier()

    eng_rr = {"i": 0}
    PREP_ONLY = (stop == "consts")

    def prep(es_bufs, W, gain, dst, KC, segs):
        stage, bstage, sbufs, bbufs_, gcol, gbuf, growT, gbufT = es_bufs
        Dout = W.shape[1]
        DoutS = dst.shape[2]
        if gain is not None:
            P.dma(growT[0:KC, :], gain.rearrange("(kc p) -> kc p", p=128), reads=[], writes=[gbufT])
            pg, pgB = ps_short()
            P.op("pe", lambda e, pg=pg, KC=KC: e.matmul(pg[:, 0:KC], lhsT=growT[0:KC, :], rhs=ident[0:KC, 0:KC], start=True, stop=True),
                 [gbufT, B_const], [pgB])
            P.op("dve", lambda e, pg=pg, KC=KC: e.tensor_copy(out=gcol[:, 0:KC], in_=pg[:, 0:KC]), [pgB], [gbuf])
        for kc in range(KC):
            r = eng_rr["i"] % 5
            eng_rr["i"] += 1
            st, bs, sB, bB = stage[r], bstage[r], sbufs[r], bbufs_[r]
            P.dma(st[:, 0:Dout], W[kc * 128:(kc + 1) * 128, :], writes=[sB])
            eng = ("dve", "act")[eng_rr["i"] % 2]
            for (s0, n, d0) in segs:
                src = st[:, s0:s0 + n]
                dd = bs[:, d0:d0 + n]
                if gain is None:
                    if eng == "act":
                        P.op("act", lambda e, dd=dd, src=src: e.copy(out=dd, in_=src), [sB], [bB])
                    else:
                        P.op(eng, lambda e, dd=dd, src=src: e.tensor_copy(out=dd, in_=src), [sB], [bB])
                else:
                    gc = gcol[:, kc:kc + 1]
                    if eng == "act":
                        P.op("act", lambda e, dd=dd, src=src, gc=gc: e.mul(out=dd, in_=src, mul=gc), [sB, gbuf], [bB])
                    else:
                        P.op(eng, lambda e, dd=dd, src=src, gc=gc: e.tensor_scalar(out=dd, in0=src, scalar1=gc, scalar2=None, op0=ALU.mult),
                             [sB, gbuf], [bB])
            P.dma(dst[:, kc, :], bs[:, 0:DoutS], reads=[bB], writes=[], q="pool")

    with ExitStack() as es:
        stage = [sbs(es, f"pst{i}", [128, 4096], F32) for i in range(5)]
        bstage = [sbs(es, f"pbs{i}", [128, 4096], BF16) for i in range(5)]
        ebufs = (stage, bstage, [Buf() for _ in range(5)], [Buf() for _ in range(5)],
                 sbs(es, "gcol", [128, 32], F32), Buf(), sbs(es, "growT", [32, 128], F32), Buf())
        win_segs = [(0, 1024, 0), (1024, 64, 1024), (1024, 64, 1088), (1664, 64, 1152), (1664, 64, 1216),
                    (1152, 512, 1280), (1088, 64, 1792), (1728, 8, 1856)]
        for l in range(0 if PREP_ONLY else 2):
            prep(ebufs, a_w_in[l], a_attn_norm[l], Wi_s[l], 8, win_segs)
            prep(ebufs, a_w_o[l], None, Wao_s[l], 8, [(0, D, 0)])
            prep(ebufs, b_w_dq[l], b_attn_norm[l], Wdq_s[l], 8, [(0, 384, 0)])
            prep(ebufs, b_w_uq[l], b_q_lora_norm[l], Wuq_s[l], 3, [(0, 1536, 0)])
            prep(ebufs, b_w_o[l], None, Wbo_s[l], 8, [(0, D, 0)])
        prep(ebufs, w_dkv, kv_norm, Wdkv_s, 8, [(0, 288, 0)])
        prep(ebufs, w_ukv, kv_lora_norm, Wukv_s, 2, [(0, 2048, 0)])
        for l in range(0 if PREP_ONLY else 4):
            prep(ebufs, mlp_w_up[l], mlp_norm[l], Wup_s[l], 8, [(0, DFF, 0)])
            prep(ebufs, mlp_w_down[l], None, Wdn_s[l], 32, [(0, D, 0)])
        P.barrier()

    def rms_featmajor(es, src, srcB, KC, Dn, dst, dstB, tag):
        sq = sbs(es, f"sq_{tag}", [128, KC, T], BF16)
        rs = sbs(es, f"rs_{tag}", [128, T], F32)
        Bsq, Brs = Buf(), Buf()
        P.op("act", lambda e: e.activation(out=sq, in_=src, func=AF.Square), [srcB], [Bsq])
        pb, pbB = ps_short()

        def mm(e):
            ins = None
            for kc in range(KC):
                ins = e.matmul(pb, lhsT=onesb, rhs=sq[:, kc, :], start=(kc == 0), stop=(kc == KC - 1))
            return ins
        P.op("pe", mm, [Bsq, B_const], [pbB])
        P.op("dve", lambda e: e.tensor_scalar(out=rs, in0=pb, scalar1=1.0 / Dn, scalar2=EPS, op0=ALU.mult, op1=ALU.add),
             [pbB], [Brs])
        P.op("act", lambda e: e.activation(out=rs, in_=rs, func=AF.Ln), [Brs], [Brs])
        P.op("act", lambda e: e.activation(out=rs, in_=rs, func=AF.Exp, scale=-0.5), [Brs], [Brs])
        P.op("dve", lambda e: e.tensor_tensor(out=dst, in0=src, in1=rs.unsqueeze(1).to_broadcast([128, KC, T]), op=ALU.mult),
             [srcB, Brs], [dstB])

    def load_w(dst, dstB, src):
        P.dma(dst, src, reads=[], writes=[dstB])

    def proj_fm(wt, wB, KC, c0, M, rhs, rhsB):
        pb, pbB = ps_short()

        def mm(e):
            ins = None
            for kc in range(KC):
                ins = e.matmul(pb[0:M, :], lhsT=wt[:, kc, c0:c0 + M], rhs=rhs[:, kc, :], start=(kc == 0), stop=(kc == KC - 1))
            return ins
        P.op("pe", mm, [wB, rhsB], [pbB])
        return pb, pbB

    def mlp_tile(layer, hT, B_h, store=None):
        with ExitStack() as es:
            xn = sbs(es, "m_xn", [128, 8, T], BF16)
            B_xn = Buf()
            uT = sbs(es, "m_uT", [128, 32, T], BF16)
            B_u = [Buf() for _ in range(32)]
            rl = [sbs(es, f"m_rl{i}", [128, T], BF16) for i in range(3)]
            B_rl = [Buf() for _ in range(3)]
            wup = [sbs(es, f"m_wup{i}", [128, 8, 512], BF16) for i in range(2)]
            B_wup = [Buf(), Buf()]
            wdn = [sbs(es, f"m_wdn{i}", [128, 32, 128], BF16) for i in range(2)]
            B_wdn = [Buf(), Buf()]
            load_w(wup[0], B_wup[0], Wup_s[layer][:, :, 0:512])
            rms_featmajor(es, hT, B_h, 8, D, xn, B_xn, "m")
            for g in range(8):
                if g + 1 < 8:
                    load_w(wup[(g + 1) % 2], B_wup[(g + 1) % 2], Wup_s[layer][:, :, (g + 1) * 512:(g + 2) * 512])
                if g == 6:
                    load_w(wdn[0], B_wdn[0], Wdn_s[layer][:, :, 0:128])
                for fi in range(4):
                    f = g * 4 + fi
                    pb, pbB = proj_fm(wup[g % 2], B_wup[g % 2], 8, fi * 128, 128, xn, B_xn)
                    r = f % 3
                    P.op("act", lambda e, pb=pb, r=r: e.activation(out=rl[r], in_=pb, func=AF.Relu), [pbB], [B_rl[r]])
                    eng = "pool" if f % 2 == 0 else "dve"
                    P.op(eng, lambda e, r=r, f=f: e.tensor_tensor(out=uT[:, f, :], in0=rl[r], in1=rl[r], op=ALU.mult),
                         [B_rl[r]], [B_u[f]])
            for o in range(8):
                if o + 1 < 8:
                    load_w(wdn[(o + 1) % 2], B_wdn[(o + 1) % 2], Wdn_s[layer][:, :, (o + 1) * 128:(o + 2) * 128])
                pb, pbB = ps_short()
                w = wdn[o % 2]

                def mm(e, pb=pb, w=w):
                    ins = None
                    for f in range(32):
                        ins = e.matmul(pb, lhsT=w[:, f, :], rhs=uT[:, f, :], start=(f == 0), stop=(f == 31))
                    return ins
                P.op("pe", mm, [B_wdn[o % 2]] + B_u, [pbB])
                P.op("dve", lambda e, pb=pb, o=o: e.tensor_tensor(out=hT[:, o, :], in0=pb, in1=hT[:, o, :], op=ALU.add),
                     [pbB, B_h], [B_h])
            if store is not None:
                store()
            P.barrier()

    acc_rot = {"i": 0}

    def attn_head(nkb, s_mm, exp_args, v_lhsT, vB, PT, B_PT, rsb, B_rs, out_ap, outB, extra_reads, post_mask=None):
        ai = acc_rot["i"] % 2
        acc_rot["i"] += 1
        acc, accB = banks[ai], bbufs[ai]
        sb_list = []

        def issue_s(kb):
            pb, pbB = ps_short()
            P.op("pe", lambda e, pb=pb, kb=kb: s_mm(e, pb, kb), extra_reads(kb), [pbB])
            sb_list.append((pb, pbB))
        issue_s(0)
        if nkb > 1:
            issue_s(1)
        for kb in range(nkb):
            if kb + 2 < nkb:
                issue_s(kb + 2)
            pb, pbB = sb_list[kb]
            r = kb % len(PT)
            bias_ap, scale = exp_args(kb)
            P.op("act", lambda e, pb=pb, r=r, bias_ap=bias_ap, scale=scale:
                 e.activation(out=PT[r], in_=pb, func=AF.Exp, bias=bias_ap, scale=scale),
                 [pbB, B_const], [B_PT[r]])
            if post_mask is not None:
                mk, mkB = post_mask(kb)
                P.op("dve", lambda e, r=r, mk=mk: e.tensor_tensor(out=PT[r], in0=PT[r], in1=mk, op=ALU.mult), [B_PT[r], mkB], [B_PT[r]])
            P.op("pe", lambda e, kb=kb, r=r: e.matmul(acc, lhsT=v_lhsT(kb), rhs=PT[r], start=(kb == 0), stop=(kb == nkb - 1)),
                 [B_PT[r], vB], [accB])
        ri = acc_rot["i"] % 2
        P.op("dve", lambda e: e.reciprocal(out=rsb[ri][64:128, :], in_=acc[64:128, :]), [accB], [B_rs[ri]])
        P.op("dve", lambda e: e.tensor_tensor(out=out_ap, in0=acc[0:64, :], in1=rsb[ri][64:128, :], op=ALU.mult),
             [accB, B_rs[ri]], [outB])

    def oproj_tile(es, Wo, attnT, B_attn, hT, B_h, tag):
        wo = sbs(es, f"wo_{tag}", [128, 8, D], BF16)
        B_wo = Buf()
        load_w(wo, B_wo, Wo)
        for o in range(8):
            pb, pbB = proj_fm(wo, B_wo, 8, o * 128, 128, attnT, B_attn)
            P.op("dve", lambda e, pb=pb, o=o: e.tensor_tensor(out=hT[:, o, :], in0=pb, in1=hT[:, o, :], op=ALU.add),
                 [pbB, B_h], [B_h])

    def dsa_layer(l, es_l, strips, B_strips, cst, first):
        kT2 = sbs(es_l, "kT2", [128, S], BF16)
        kiT2 = sbs(es_l, "kiT2", [128, S], BF16)
        Vaug = sbs(es_l, "Vaug", [128, 32, 128], BF16)
        gq = sbs(es_l, "gq", [128, 2], F32)
        B_k, B_ki, B_V_, B_g = Buf(), Buf(), Buf(), Buf()
        P.op("pool", lambda e: e.memset(Vaug[:, :, 64:128], 1.0), [], [B_V_])
        for hf in range(2):
            P.dma(gq[hf * 64:(hf + 1) * 64, 0:1], a_q_norm[l].rearrange("(p o) -> p o", o=1), writes=[B_g], allow_slow_non_contiguous=True)
            P.dma(gq[hf * 64:(hf + 1) * 64, 1:2], a_k_norm[l].rearrange("(p o) -> p o", o=1), writes=[B_g], allow_slow_non_contiguous=True)
        P.op("dve", lambda e: e.tensor_scalar(out=gq[:, 0:1], in0=gq[:, 0:1], scalar1=0.125, scalar2=None, op0=ALU.mult), [B_g], [B_g])
        CW = 1.0 / (8.0 * math.sqrt(8.0))
        for j in range(NT):
            t0 = j * T
            W = (j + 1) * T
            with ExitStack() as es_t:
                hT = sbs(es_t, "hT", [128, 8, T], F32)
                B_h = Buf()
                src = xT if first else Hs
                P.dma(hT, src.rearrange("kc p t -> p kc t")[:, :, t0:t0 + T], reads=[B_Hs[j]], writes=[B_h])
                es_a = ExitStack()
                qT = sbs(es_a, "qT", [128, 8, T], BF16)
                qiT = sbs(es_a, "qiT", [128, 4, T], BF16)
                wtok = sbs(es_a, "wtok", [128, 4, 8], F32)
                attnT = sbs(es_a, "attnT", [128, 8, T], BF16)
                maskb = sbs(es_a, "maskb", [128, 4, S], BF16)
                B_q, B_qi, B_w, B_attn = Buf(), Buf(), Buf(), Buf()
                B_mb = [Buf() for _ in range(4)]
                with ExitStack() as es:
                    xn = sbs(es, "xn", [128, 8, T], BF16)
                    B_xn = Buf()
                    wi = sbs(es, "wi", [128, 8, WIC], BF16)
                    B_wi = Buf()
                    load_w(wi, B_wi, Wi_s[l])
                    rms_featmajor(es, hT, B_h, 8, D, xn, B_xn, "a")
                    sq2 = [sbs(es, f"sq2_{i}", [128, T], BF16) for i in range(2)]
                    rs2 = [sbs(es, f"rs2_{i}", [128, T], F32) for i in range(2)]
                    B_sq2 = [Buf(), Buf()]
                    B_rs2 = [Buf(), Buf()]
                    for c in range(14):
                        pb, pbB = proj_fm(wi, B_wi, 8, c * 128, 128, xn, B_xn)
                        if c < 9:
                            r = c % 2
                            P.op("act", lambda e, pb=pb, r=r: e.activation(out=sq2[r], in_=pb, func=AF.Square), [pbB], [B_sq2[r]])
                            p2, p2B = ps_short()
                            P.op("pe", lambda e, p2=p2, r=r: e.matmul(p2, lhsT=blk2b, rhs=sq2[r], start=True, stop=True),
                                 [B_sq2[r], B_const], [p2B])
                            P.op("dve", lambda e, p2=p2, r=r: e.tensor_scalar(out=rs2[r], in0=p2, scalar1=1.0 / 64, scalar2=EPS, op0=ALU.mult, op1=ALU.add),
                                 [p2B], [B_rs2[r]])
                            P.op("act", lambda e, r=r: e.activation(out=rs2[r], in_=rs2[r], func=AF.Ln), [B_rs2[r]], [B_rs2[r]])
                            P.op("act", lambda e, r=r: e.activation(out=rs2[r], in_=rs2[r], func=AF.Exp, scale=-0.5), [B_rs2[r]], [B_rs2[r]])
                            if c < 8:
                                dst, dB, gc = qT[:, c, :], B_q, gq[:, 0:1]
                            else:
                                dst, dB, gc = kT2[:, t0:t0 + T], B_k, gq[:, 1:2]
                            P.op("dve", lambda e, pb=pb, r=r, dst=dst, gc=gc: e.scalar_tensor_tensor(out=dst, in0=pb, scalar=gc, in1=rs2[r], op0=ALU.mult, op1=ALU.mult),
                                 [pbB, B_rs2[r], B_g], [dB])
                        elif c == 9:
                            P.op("act", lambda e, pb=pb: e.copy(out=kiT2[:, t0:t0 + T], in_=pb), [pbB], [B_ki])
                        else:
                            P.op("act", lambda e, pb=pb, c=c: e.copy(out=qiT[:, c - 10, :], in_=pb), [pbB], [B_qi])
                    for tb in range(4):
                        pb, pbB = ps_short()

                        def mm(e, pb=pb, tb=tb):
                            ins = None
                            for kc in range(8):
                                ins = e.matmul(pb[:, 0:72], lhsT=xn[:, kc, tb * 128:(tb + 1) * 128], rhs=wi[:, kc, 1792:1864],
                                               start=(kc == 0), stop=(kc == 7))
                            return ins
                        P.op("pe", mm, [B_xn, B_wi], [pbB])
                        P.op("dve", lambda e, pb=pb, tb=tb: e.tensor_copy(out=Vaug[:, 4 * j + tb, 0:64], in_=pb[:, 0:64]), [pbB], [B_V_])
                        P.op("dve", lambda e, pb=pb, tb=tb: e.tensor_copy(out=wtok[:, tb, :], in_=pb[:, 64:72]), [pbB], [B_w])
                    P.barrier()
                ckpt(f"proj{j}")
                with ExitStack() as es:
                    score4 = sbs(es, "score4", [128, 4, S], F32)
                    B_sc = [Buf() for _ in range(4)]
                    Rb = [sbs(es, f"Rb{i}", [128, T], BF16) for i in range(4)]
                    B_R = [Buf() for _ in range(4)]
                    Dg4 = sbs(es, "Dg4", [128, 32, 128], BF16)
                    aw4 = sbs(es, "aw4", [128, 32], F32)
                    sg4 = sbs(es, "sg4", [128, 32], F32)
                    st4 = sbs(es, "st4", [128, 8, 4], F32)
                    thr4 = sbs(es, "thr4", [128, 4], F32)
                    halves4 = sbs(es, "halves4", [128, 4, NSTEP], F32)
                    B_aw, B_Dg, B_lo, B_mid, B_nm, B_cD, B_cA, B_thr = [Buf() for _ in range(8)]
                    wt2 = wtok.rearrange("p i h -> p (i h)")
                    P.op("act", lambda e: e.activation(out=aw4, in_=wt2, func=AF.Abs, scale=CW), [B_w], [B_aw])
                    P.op("act", lambda e: e.activation(out=sg4, in_=wt2, func=AF.Sign), [B_w], [B_aw])
                    P.op("dve", lambda e: e.tensor_tensor(out=Dg4, in0=identb.unsqueeze(1).to_broadcast([128, 32, 128]),
                                                           in1=sg4.unsqueeze(2).to_broadcast([128, 32, 128]), op=ALU.mult),
                         [B_aw, B_const], [B_Dg])
                    P.op("pool", lambda e: e.memset(thr4[:, 0:2], 255.5), [], [B_thr])
                    P.op("pool", lambda e: e.memset(thr4[:, 2:4], 511.0 - W), [], [B_thr])
                    for i in range(4):
                        for kt in range(j + 1):
                            ai = acc_rot["i"] % 2
                            acc_rot["i"] += 1
                            acc, accB = banks[ai], bbufs[ai]
                            dl = []

                            def issue_d(h8, kt=kt, i=i):
                                pb, pbB = ps_short()
                                hp = (h8 % 2) * 64
                                P.op("pe", lambda e, pb=pb: e.matmul(pb, lhsT=qiT[hp:hp + 64, h8 // 2, i * 128:(i + 1) * 128],
                                                                     rhs=kiT2[hp:hp + 64, kt * T:(kt + 1) * T], start=True, stop=True),
                                     [B_qi, B_ki], [pbB])
                                dl.append((pb, pbB))
                            issue_d(0)
                            issue_d(1)
                            for h8 in range(8):
                                if h8 + 2 < 8:
                                    issue_d(h8 + 2)
                                pb, pbB = dl[h8]
                                r = h8 % 4
                                awc = aw4[:, i * 8 + h8:i * 8 + h8 + 1]
                                if h8 % 2 == 0:
                                    P.op("act", lambda e, pb=pb, r=r, awc=awc: e.activation(out=Rb[r], in_=pb, func=AF.Relu, scale=awc),
                                         [pbB, B_aw], [B_R[r]])
                                else:
                                    P.op("dve", lambda e, pb=pb, r=r, awc=awc: e.tensor_scalar(out=Rb[r], in0=pb, scalar1=awc, scalar2=0.0, op0=ALU.mult, op1=ALU.max),
                                         [pbB, B_aw], [B_R[r]])
                                P.op("pe", lambda e, r=r, h8=h8, acc=acc, i=i: e.matmul(acc, lhsT=Dg4[:, i * 8 + h8, :], rhs=Rb[r], start=(h8 == 0), stop=(h8 == 7)),
                                     [B_R[r], B_Dg], [accB])
                            P.op("act", lambda e, acc=acc, kt=kt, i=i: e.copy(out=score4[:, i, kt * T:(kt + 1) * T], in_=acc), [accB], [B_sc[i]])
                    P.op("dve", lambda e: e.tensor_reduce(out=st4[:, 0, :], in_=score4[:, :, 0:W], axis=AX.X, op=ALU.max, apply_absolute_value=True),
                         B_sc, [B_lo])
                    P.op("dve", lambda e: e.tensor_scalar(out=st4[:, 1, :], in0=st4[:, 0, :], scalar1=-1.001, scalar2=-1e-6, op0=ALU.mult, op1=ALU.add),
                         [B_lo], [B_lo])
                    P.op("dve", lambda e: e.tensor_scalar(out=st4[:, 2, :], in0=st4[:, 1, :], scalar1=-2.0, scalar2=None, op0=ALU.mult),
                         [B_lo], [B_lo])
                    P.op("dve", lambda e: e.tensor_tensor(out=halves4, in0=pw.unsqueeze(1).to_broadcast([128, 4, NSTEP]),
                                                           in1=st4[:, 2, :].unsqueeze(2).to_broadcast([128, 4, NSTEP]), op=ALU.mult),
                         [B_lo, B_const], [B_lo])
                    for i in range(4):
                        n_adm = (4 * j + i + 1) * 128
                        if n_adm - 64 < W:
                            P.op("pool", lambda e, n_adm=n_adm, i=i: e.memset(score4[0:64, i, n_adm - 64:W], -1e30), [B_lo], [B_sc[i]])
                        if n_adm < W:
                            P.op("pool", lambda e, n_adm=n_adm, i=i: e.memset(score4[64:128, i, n_adm:W], -1e30), [B_lo], [B_sc[i]])
                    for s_ in range(NSTEP):
                        hs = halves4[:, :, s_]
                        P.op("dve", lambda e, hs=hs: e.tensor_tensor(out=st4[:, 3, :], in0=st4[:, 1, :], in1=hs, op=ALU.add), [B_lo], [B_mid])
                        P.op("dve", lambda e: e.tensor_scalar(out=st4[:, 7, 2:4], in0=st4[:, 3, 2:4], scalar1=-1.0, scalar2=None, op0=ALU.mult),
                             [B_mid], [B_nm])
                        P.op("pool", lambda e: e.memset(st4[:, 4, 2:4], 0.0), [], [B_cA])
                        for i in range(2):
                            P.op("dve", lambda e, i=i: e.tensor_scalar(out=maskb[:, i, 0:W], in0=score4[:, i, 0:W], scalar1=st4[:, 3, i:i + 1], scalar2=None,
                                                                      op0=ALU.is_gt, op1=ALU.add, accum_out=st4[:, 4, i:i + 1]),
                                 [B_sc[i], B_mid], [B_mb[i], B_cD])
                        for i in range(2, 4):
                            P.op("act", lambda e, i=i: e.activation(out=maskb[:, i, 0:W], in_=score4[:, i, 0:W], func=AF.Sign, bias=st4[:, 7, i:i + 1],
                                                                   scale=1.0, accum_out=st4[:, 4, i:i + 1]),
                                 [B_sc[i], B_nm], [B_mb[i], B_cA])
                        P.op("dve", lambda e: e.tensor_tensor(out=st4[:, 5, :], in0=st4[:, 4, :], in1=thr4, op=ALU.is_ge), [B_cD, B_cA, B_thr], [B_lo])
                        P.op("dve", lambda e, hs=hs: e.tensor_tensor(out=st4[:, 6, :], in0=st4[:, 5, :], in1=hs, op=ALU.mult), [B_lo], [B_lo])
                        P.op("dve", lambda e: e.tensor_tensor(out=st4[:, 1, :], in0=st4[:, 1, :], in1=st4[:, 6, :], op=ALU.add), [B_lo], [B_lo])
                    for i in range(4):
                        P.op("dve", lambda e, i=i: e.tensor_scalar(out=maskb[:, i, 0:W], in0=score4[:, i, 0:W], scalar1=st4[:, 1, i:i + 1], scalar2=NEGM,
                                                                  op0=ALU.is_le, op1=ALU.mult),
                             [B_sc[i], B_lo], [B_mb[i]])
                    P.barrier()
                ckpt(f"idx{j}")
                with ExitStack() as es:
                    nkb = 4 * j + 4
                    maskT = sbs(es, "maskT", [128, 32, T], BF16)
                    B_mT = [Buf() for _ in range(32)]
                    PT = [sbs(es, f"PT{i}", [128, T], BF16) for i in range(4)]
                    B_PT = [Buf() for _ in range(4)]
                    rsb = [sbs(es, f"rsb{i}", [128, T], F32) for i in range(2)]
                    B_rs = [Buf(), Buf()]
                    for kb in range(nkb):
                        pt, ptB = ps_tr()

                        def tr(e, pt=pt, kb=kb):
                            ins = None
                            for i in range(4):
                                ins = e.transpose(pt[:, i * 128:(i + 1) * 128], maskb[:, i, kb * 128:(kb + 1) * 128], identb)
                            return ins
                        P.op("pe", tr, B_mb + [B_const], [ptB])
                        import os
                        if os.environ.get("KDBG") == "nocopy":
                            continue
                        eng = "act" if kb % 2 == 0 else "dve"
                        if eng == "act":
                            P.op("act", lambda e, pt=pt, kb=kb: e.copy(out=maskT[:, kb, :], in_=pt), [ptB], [B_mT[kb]])
                        else:
                            P.op("dve", lambda e, pt=pt, kb=kb: e.tensor_copy(out=maskT[:, kb, :], in_=pt), [ptB], [B_mT[kb]])
                    ckpt(f"tr{j}")
                    for h in range(16):
                        ckpt(f"head{j}_{h}")
                        hp = (h % 2) * 64
                        c = h // 2

                        def s_mm(e, pb, kb, hp=hp, c=c, h=h):
                            near = kb >= 4 * j - 1
                            e.matmul(pb, lhsT=kT2[hp:hp + 64, kb * 128:(kb + 1) * 128], rhs=qT[hp:hp + 64, c, :], start=True, stop=False)
                            ins = e.matmul(pb, lhsT=identb, rhs=maskT[:, kb, :], start=False, stop=(not near))
                            if near:
                                off = (3 - (kb - 4 * j)) * 128
                                ins = e.matmul(pb, lhsT=identb, rhs=strips[:, h, off:off + T], start=False, stop=True)
                            return ins

                        def exp_args(kb, h=h):
                            if kb >= 4 * j - 1:
                                return zeros[:, 0:1], 1.0
                            return cst[:, h:h + 1], 1.0

                        attn_head(nkb, s_mm, exp_args, lambda kb: Vaug[:, kb, :], B_V_, PT, B_PT, rsb, B_rs,
                                  attnT[hp:hp + 64, c, :], B_attn,
                                  lambda kb: [B_k, B_q, B_mT[kb], B_strips, B_const])
                    P.barrier()
                ckpt(f"attn{j}")
                with ExitStack() as es:
                    oproj_tile(es, Wao_s[l], attnT, B_attn, hT, B_h, "a")
                    P.barrier()
                es_a.close()
                ckpt(f"oproj{j}")
                dstH = outT if (l == n_layers - 1) else Hs
                mlp_tile(l, hT, B_h, store=lambda: P.dma(dstH.rearrange("kc p t -> p kc t")[:, :, t0:t0 + T], hT, reads=[B_h], writes=[B_Hs[j]]))
                ckpt(f"mlp{j}")

    def qk_tmps(es, tag):
        sq = sbs(es, f"qk_sq_{tag}", [128, 16, 96], F32)
        ss = sbs(es, f"qk_ss_{tag}", [128, 16], F32)
        t1 = sbs(es, f"qk_t1_{tag}", [128, 16, 32], F32)
        t2 = sbs(es, f"qk_t2_{tag}", [128, 16, 32], F32)
        return (sq, ss, t1, t2, Buf(), Buf(), Buf(), Buf())

    def qk_finish(tm, full, B_full, gain_b, cs, sn, B_cs, outb, B_out):
        sq, ss, t1, t2, Bs, Bss, Bt1, Bt2 = tm
        P.op("act", lambda e: e.activation(out=sq, in_=full, func=AF.Square), [B_full], [Bs])
        P.op("dve", lambda e: e.tensor_reduce(out=ss, in_=sq, axis=AX.X, op=ALU.add), [Bs], [Bss])
        P.op("dve", lambda e: e.tensor_scalar(out=ss, in0=ss, scalar1=1.0 / 96, scalar2=EPS, op0=ALU.mult, op1=ALU.add), [Bss], [Bss])
        P.op("act", lambda e: e.activation(out=ss, in_=ss, func=AF.Ln), [Bss], [Bss])
        P.op("act", lambda e: e.activation(out=ss, in_=ss, func=AF.Exp, scale=-0.5), [Bss], [Bss])
        P.op("dve", lambda e: e.tensor_tensor(out=full, in0=full, in1=ss.unsqueeze(2).to_broadcast([128, 16, 96]), op=ALU.mult),
             [B_full, Bss], [B_full])
        P.op("dve", lambda e: e.tensor_tensor(out=full, in0=full, in1=gain_b.unsqueeze(1).to_broadcast([128, 16, 96]), op=ALU.mult),
             [B_full, B_const], [B_full])
        P.op("pool", lambda e: e.tensor_copy(out=outb[:, :, 0:64], in_=full[:, :, 0:64]), [B_full], [B_out])
        P.op("dve", lambda e: e.tensor_tensor(out=t1, in0=full[:, :, 64:96], in1=cs.unsqueeze(1).to_broadcast([128, 16, 32]), op=ALU.mult),
             [B_full, B_cs], [Bt1])
        P.op("dve", lambda e: e.tensor_tensor(out=t2[:, :, 0:16], in0=full[:, :, 80:96], in1=sn[:, 0:16].unsqueeze(1).to_broadcast([128, 16, 16]), op=ALU.mult),
             [B_full, B_cs], [Bt2])
        P.op("dve", lambda e: e.tensor_tensor(out=t2[:, :, 16:32], in0=full[:, :, 64:80], in1=sn[:, 16:32].unsqueeze(1).to_broadcast([128, 16, 16]), op=ALU.mult),
             [B_full, B_cs, Bt2], [Bt2])
        P.op("dve", lambda e: e.tensor_tensor(out=outb[:, :, 64:96], in0=t1, in1=t2, op=ALU.add), [Bt1, Bt2, B_out], [B_out])

    def lat_norm(es, src_ps_list, KC, Dn, dst, dstB, tag):
        raw = sbs(es, f"ln_raw_{tag}", [128, KC, T], F32)
        Braw = Buf()
        for kc, (pb, pbB) in enumerate(src_ps_list):
            P.op("act", lambda e, pb=pb, kc=kc: e.copy(out=raw[:, kc, :], in_=pb), [pbB], [Braw])
        rms_featmajor(es, raw, Braw, KC, Dn, dst, dstB, tag)

    def mla_layer(lb, layer, es_l, gen_kv, gk_b, gq_b):
        for j in range(NT):
            t0 = j * T
            with ExitStack() as es_t:
                hT = sbs(es_t, "hT", [128, 8, T], F32)
                B_h = Buf()
                P.dma(hT, Hs.rearrange("kc p t -> p kc t")[:, :, t0:t0 + T], reads=[B_Hs[j]], writes=[B_h])
                qT = sbs(es_t, "b_qT", [128, 16, T], BF16)
                B_qT = Buf()
                attnT = sbs(es_t, "attnT", [128, 8, T], BF16)
                B_attn = Buf()
                cs = sbs(es_t, "cs", [128, 4, 32], F32)
                sn = sbs(es_t, "sn", [128, 4, 32], F32)
                B_cs = Buf()
                P.dma(cs, c_cos[t0:t0 + T, :].rearrange("(tb p) d -> p tb d", p=128), writes=[B_cs])
                P.dma(sn, c_sinm[t0:t0 + T, :].rearrange("(tb p) d -> p tb d", p=128), writes=[B_cs])
                if gen_kv:
                    with ExitStack() as es:
                        xn = sbs(es, "xn", [128, 8, T], BF16)
                        B_xn = Buf()
                        wd = sbs(es, "wdkv", [128, 8, 288], BF16)
                        wu = sbs(es, "wukv", [128, 2, 2048], BF16)
                        B_wd, B_wu = Buf(), Buf()
                        load_w(wd, B_wd, Wdkv_s)
                        load_w(wu, B_wu, Wukv_s)
                        rms_featmajor(es, hT, B_h, 8, D, xn, B_xn, "kv")
                        lat = sbs(es, "lat", [128, 2, T], BF16)
                        B_lat = Buf()
                        pl = [proj_fm(wd, B_wd, 8, c * 128, 128, xn, B_xn) for c in range(2)]
                        lat_norm(es, pl, 2, 256, lat, B_lat, "lat")
                        KTst = sbs(es, "KTst", [128, 16, T], BF16)
                        B_KTst = Buf()
                        kfull = [sbs(es, f"kfull{i}", [128, 16, 96], F32) for i in range(2)]
                        kb16 = [sbs(es, f"kb16{i}", [128, 16, 96], BF16) for i in range(2)]
                        vtok = [sbs(es, f"vtok{i}", [128, 16, 64], BF16) for i in range(2)]
                        B_kf = [Buf(), Buf()]
                        B_kb = [Buf(), Buf()]
                        B_vt = [Buf(), Buf()]
                        ktm = qk_tmps(es, "k")
                        for tb in range(4):
                            r = tb % 2
                            pr, prB = ps_short()

                            def mmr(e, pr=pr, tb=tb):
                                ins = None
                                for kc in range(8):
                                    ins = e.matmul(pr[:, 0:32], lhsT=xn[:, kc, tb * 128:(tb + 1) * 128], rhs=wd[:, kc, 256:288],
                                                   start=(kc == 0), stop=(kc == 7))
                                return ins
                            P.op("pe", mmr, [B_xn, B_wd], [prB])
                            P.op("dve", lambda e, pr=pr, r=r: e.tensor_copy(out=kfull[r][:, :, 64:96], in_=pr[:, 0:32].unsqueeze(1).to_broadcast([128, 16, 32])),
                                 [prB], [B_kf[r]])
                            for cg in range(4):
                                pk, pkB = ps_short()

                                def mmk(e, pk=pk, tb=tb, cg=cg):
                                    ins = None
                                    for c2 in range(2):
                                        ins = e.matmul(pk, lhsT=lat[:, c2, tb * 128:(tb + 1) * 128], rhs=wu[:, c2, cg * 512:(cg + 1) * 512],
                                                       start=(c2 == 0), stop=(c2 == 1))
                                    return ins
                                P.op("pe", mmk, [B_lat, B_wu], [pkB])
                                pk3 = pk.rearrange("p (h d) -> p h d", h=4)
                                if cg % 2 == 0:
                                    P.op("act", lambda e, pk3=pk3, r=r, cg=cg: e.copy(out=kfull[r][:, 4 * cg:4 * cg + 4, 0:64], in_=pk3[:, :, 0:64]),
                                         [pkB], [B_kf[r]])
                                    P.op("act", lambda e, pk3=pk3, r=r, cg=cg: e.copy(out=vtok[r][:, 4 * cg:4 * cg + 4, :], in_=pk3[:, :, 64:128]),
                                         [pkB], [B_vt[r]])
                                else:
                                    P.op("dve", lambda e, pk3=pk3, r=r, cg=cg: e.tensor_copy(out=kfull[r][:, 4 * cg:4 * cg + 4, 0:64], in_=pk3[:, :, 0:64]),
                                         [pkB], [B_kf[r]])
                                    P.op("dve", lambda e, pk3=pk3, r=r, cg=cg: e.tensor_copy(out=vtok[r][:, 4 * cg:4 * cg + 4, :], in_=pk3[:, :, 64:128]),
                                         [pkB], [B_vt[r]])
                            P.dma(V_s[:, :, 4 * j + tb, :].rearrange("h p d -> p h d"), vtok[r], reads=[B_vt[r]], writes=[B_V[j]])
                            qk_finish(ktm, kfull[r], B_kf[r], gk_b, cs[:, tb, :], sn[:, tb, :], B_cs, kb16[r], B_kb[r])
                            for hg in range(4):
                                pt, ptB = ps_tr()

                                def tr(e, pt=pt, r=r, hg=hg):
                                    ins = None
                                    for hh in range(4):
                                        ins = e.transpose(pt[0:96, hh * 128:(hh + 1) * 128], kb16[r][:, 4 * hg + hh, :], identb)
                                    return ins
                                P.op("pe", tr, [B_kb[r], B_const], [ptB])
                                pt3 = pt.rearrange("p (h t) -> p h t", h=4)
                                P.op("act", lambda e, pt3=pt3, hg=hg, tb=tb: e.copy(out=KTst[0:96, 4 * hg:4 * hg + 4, tb * 128:(tb + 1) * 128], in_=pt3[0:96, :, :]),
                                     [ptB], [B_KTst])
                        P.dma(KT_s[:, :, t0:t0 + T].rearrange("h d t -> d h t"), KTst[0:96, :, :], reads=[B_KTst], writes=[B_KT[j]])
                        P.barrier()
                ckpt(f"kv{j}")
                with ExitStack() as es:
                    xn = sbs(es, "xn", [128, 8, T], BF16)
                    B_xn = Buf()
                    wd = sbs(es, "wdq", [128, 8, 384], BF16)
                    wu = sbs(es, "wuq", [128, 3, 1536], BF16)
                    B_wd, B_wu = Buf(), Buf()
                    load_w(wd, B_wd, Wdq_s[lb])
                    load_w(wu, B_wu, Wuq_s[lb])
                    rms_featmajor(es, hT, B_h, 8, D, xn, B_xn, "q")
                    cq = sbs(es, "cq", [128, 3, T], BF16)
                    B_cq = Buf()
                    pl = [proj_fm(wd, B_wd, 8, c * 128, 128, xn, B_xn) for c in range(3)]
                    lat_norm(es, pl, 3, 384, cq, B_cq, "cq")
                    qfull = [sbs(es, f"qfull{i}", [128, 16, 96], F32) for i in range(2)]
                    qb16 = [sbs(es, f"qb16{i}", [128, 16, 96], BF16) for i in range(2)]
                    B_qf = [Buf(), Buf()]
                    B_qb = [Buf(), Buf()]
                    qtm = qk_tmps(es, "q")
                    for tb in range(4):
                        r = tb % 2
                        for cg in range(3):
                            pq, pqB = ps_short()

                            def mmq(e, pq=pq, tb=tb, cg=cg):
                                ins = None
                                for c3 in range(3):
                                    ins = e.matmul(pq, lhsT=cq[:, c3, tb * 128:(tb + 1) * 128], rhs=wu[:, c3, cg * 512:(cg + 1) * 512],
                                                   start=(c3 == 0), stop=(c3 == 2))
                                return ins
                            P.op("pe", mmq, [B_cq, B_wu], [pqB])
                            qf2 = qfull[r].rearrange("p h d -> p (h d)")
                            P.op("act", lambda e, pq=pq, qf2=qf2, cg=cg: e.copy(out=qf2[:, cg * 512:(cg + 1) * 512], in_=pq), [pqB], [B_qf[r]])
                        qk_finish(qtm, qfull[r], B_qf[r], gq_b, cs[:, tb, :], sn[:, tb, :], B_cs, qb16[r], B_qb[r])
                        for hg in range(4):
                            pt, ptB = ps_tr()

                            def tr(e, pt=pt, r=r, hg=hg):
                                ins = None
                                for hh in range(4):
                                    ins = e.transpose(pt[0:96, hh * 128:(hh + 1) * 128], qb16[r][:, 4 * hg + hh, :], identb)
                                return ins
                            P.op("pe", tr, [B_qb[r], B_const], [ptB])
                            pt3 = pt.rearrange("p (h t) -> p h t", h=4)
                            P.op("dve", lambda e, pt3=pt3, hg=hg, tb=tb: e.tensor_copy(out=qT[0:96, 4 * hg:4 * hg + 4, tb * 128:(tb + 1) * 128], in_=pt3[0:96, :, :]),
                                 [ptB], [B_qT])
                    P.barrier()
                ckpt(f"q{j}")
                with ExitStack() as es:
                    nkb = 4 * j + 4
                    nk = nkb * 128
                    KTh = [sbs(es, f"KTh{i}", [128, S], BF16) for i in range(2)]
                    Vh = [sbs(es, f"Vh{i}", [128, 32, 128], BF16) for i in range(2)]
                    B_KTh = [Buf(), Buf()]
                    B_Vh = [Buf(), Buf()]
                    PT = [sbs(es, f"PT{i}", [128, T], BF16) for i in range(4)]
                    B_PT = [Buf() for _ in range(4)]
                    rsb = [sbs(es, f"rsb{i}", [128, T], F32) for i in range(2)]
                    B_rs = [Buf(), Buf()]
                    for i in range(2):
                        P.op("pool", lambda e, i=i: e.memset(Vh[i][:, :, 64:128], 1.0), [], [B_Vh[i]])

                    def load_head(h):
                        r = h % 2
                        P.dma(KTh[r][0:96, 0:nk], KT_s[h, :, 0:nk], reads=B_KT[0:j + 1], writes=[B_KTh[r]])
                        P.dma(Vh[r][:, 0:nkb, 0:64], V_s[h, :, 0:nkb, :], reads=B_V[0:j + 1], writes=[B_Vh[r]])
                    load_head(0)
                    SC = 96 ** -0.5
                    for h in range(16):
                        if h + 1 < 16:
                            load_head(h + 1)
                        r = h % 2
                        hp = (h % 2) * 64
                        c = h // 2

                        def s_mm(e, pb, kb, h=h, r=r):
                            near = kb >= 4 * j
                            ins = e.matmul(pb, lhsT=KTh[r][0:96, kb * 128:(kb + 1) * 128], rhs=qT[0:96, h, :], start=True, stop=(not near))
                            if near:
                                off = (3 - (kb - 4 * j)) * 128
                                ins = e.matmul(pb, lhsT=identb, rhs=mstrip[:, off:off + T], start=False, stop=True)
                            return ins

                        attn_head(nkb, s_mm, lambda kb: (zeros[:, 0:1], SC), lambda kb, r=r: Vh[r][:, kb, :], B_Vh[r], PT, B_PT, rsb, B_rs,
                                  attnT[hp:hp + 64, c, :], B_attn,
                                  lambda kb, r=r: [B_KTh[r], B_qT, B_const])
                    P.barrier()
                ckpt(f"battn{j}")
                with ExitStack() as es:
                    oproj_tile(es, Wbo_s[lb], attnT, B_attn, hT, B_h, "b")
                    P.barrier()
                ckpt(f"boproj{j}")
                last = (layer == n_layers - 1)
                dstH = outT if last else Hs
                mlp_tile(layer, hT, B_h, store=lambda: P.dma(dstH.rearrange("kc p t -> p kc t")[:, :, t0:t0 + T], hT, reads=[B_h], writes=[B_Hs[j]]))

    try:
        nA = min(2, n_layers)
        with ExitStack() as es_dsa:
            strips = sbs(es_dsa, "strips", [128, 16, 1024], BF16)
            cst = sbs(es_dsa, "cst", [128, 16], F32)
            B_strips = Buf()
            with ExitStack() as es:
                rb = sbs(es, "rb", [32, 16], F32)
                oh = sbs(es, "oh", [32, 1152], F32)
                gt = sbs(es, "gt", [16, 1152], F32)
                wn = sbs(es, "wn", [128, 1024], F32)
                mst = sbs(es, "mst", [128, 1024], F32)
                Brb, Bgt, Bwn, Bmst = Buf(), Buf(), Buf(), Buf()
                P.dma(rb, rel_bias, writes=[Brb])
                P.dma(oh, c_oh, writes=[Brb])
                P.dma(mst, c_mstrip, writes=[Bmst])
                for cch in range(3):
                    pb, pbB = ps_short()
                    P.op("pe", lambda e, pb=pb, cch=cch: e.matmul(pb[0:16, 0:384], lhsT=rb, rhs=oh[:, cch * 384:(cch + 1) * 384], start=True, stop=True),
                         [Brb], [pbB])
                    P.op("dve", lambda e, pb=pb, cch=cch: e.tensor_copy(out=gt[:, cch * 384:(cch + 1) * 384], in_=pb[0:16, 0:384]), [pbB], [Bgt])
                P.dma(G_s, gt, reads=[Bgt], writes=[B_G])
                for h in range(16):
                    wap = bass.AP(G_s.tensor, h * 1152, [[1, 128], [1, 1024]])
                    P.dma(wn, wap, reads=[B_G], writes=[Bwn])
                    wrev = bass.AP(wn.tensor, wn.offset + 1023, [list(wn.ap[0]), [-1, 1024]])
                    P.op("dve", lambda e, wrev=wrev, h=h: e.tensor_tensor(out=strips[:, h, :], in0=wrev, in1=mst, op=ALU.add),
                         [Bwn, Bmst], [B_strips])
                    P.op("dve", lambda e, h=h: e.tensor_copy(out=cst[:, h:h + 1], in_=wn[:, 0:1]), [Bwn], [B_strips])
                P.barrier()
            for l in range(nA):
                with ExitStack() as es_l:
                    ckpt("strips")
                    ckpt("consts")
                    dsa_layer(l, es_l, strips, B_strips, cst, first=(l == 0))
                    P.barrier()
            P.barrier()
        if n_layers > 2:
            gk_b = sbp("gk_b", [128, 96], F32)
            gq_bs = [sbp(f"gq_b{i}", [128, 96], F32) for i in range(2)]
            P.dma(gk_b, bass.AP(k_norm.tensor, 0, [[0, 128], [1, 96]]), writes=[B_const])
            for i in range(2):
                P.dma(gq_bs[i], bass.AP(b_q_norm.tensor, i * 96, [[0, 128], [1, 96]]), writes=[B_const])
            for lb in range(n_layers - 2):
                with ExitStack() as es_l:
                    mla_layer(lb, 2 + lb, es_l, gen_kv=(lb == 0), gk_b=gk_b, gq_b=gq_bs[lb])
                    P.barrier()
    except _Stop:
        pass
    P.barrier()
    P.emit()
    return nc


_CACHE = {}


def kernel(**inputs):
    n_layers = 4
    if "nc" not in _CACHE:
        _CACHE["nc"] = build(n_layers)
        _CACHE["consts"] = _consts()
    nc = _CACHE["nc"]
    cs = _CACHE["consts"]
    x = np.asarray(inputs["x"], dtype=np.float32)
    shared = {k: np.ascontiguousarray(np.asarray(v, dtype=np.float32)) for k, v in inputs.items() if k != "x"}
    shared.update({"c_ident": cs["ident"], "c_blk2": cs["blk2"], "c_oh": cs["oh"], "c_mstrip": cs["mstrip"],
                   "c_cos": cs["cos"], "c_sinm": cs["sinm"], "c_pw": cs["pw"]})
    in_maps = []
    for b in range(8):
        m = dict(shared)
        m["xT"] = np.ascontiguousarray(x[b].T).reshape(8, 128, S)
        in_maps.append(m)
    res = run_bass_kernel_spmd(nc, in_maps, core_ids=list(range(8)))
    out = np.stack([np.ascontiguousarray(r["outT"].reshape(D, S).T) for r in res.results], 0)
    return out.astype(np.float32)
```
